# Optimizing a Trainium2 kernel written in Bass

```python
import jax, jax.numpy as jnp
from jax import lax
import numpy as np

D_MODEL = 1024
BATCH = 8
SEQ = 8192
DEPTH = 1

SBA_HEADS = 8
SBA_HEAD_DIM = 64
SBA_WIDTH = SBA_HEADS * SBA_HEAD_DIM
Q_BLOCK = 128
SGU_GROUPS = 8
SGU_GROUP_DIM = 64
SGU_WIDTH = SGU_GROUPS * SGU_GROUP_DIM
CHUNK = 128
N_BRANCHES = 2
IN_COLS = 3 * SBA_WIDTH + 2 * SGU_WIDTH + N_BRANCHES * D_MODEL
N_GROUPS = 4
EXPERTS_PER_GROUP = 8
N_EXPERTS = N_GROUPS * EXPERTS_PER_GROUP
TOP_K = 2
D_EXPERT = D_MODEL // 2
EXPERT_BLOCK = 128
N_MOD = 6
EPS = 1e-6

kernel_name = "hybrid_stickbreak_sgu_hmoe_block"


def rms_norm(x, g):
    xf = x.astype(jnp.float32)
    y = xf * lax.rsqrt(jnp.mean(xf * xf, axis=-1, keepdims=True) + EPS)
    return y.astype(x.dtype) * g


def modulate(xn, shift, scale):
    return xn * (1 + scale[:, None, :]) + shift[:, None, :]


def stick_breaking_attention(q, k, v):
    B, H, S, dh = q.shape
    nb = S // Q_BLOCK
    qb = q.reshape(B, H, nb, Q_BLOCK, dh).transpose(2, 0, 1, 3, 4)
    kf = k.astype(jnp.float32)
    kpos = jnp.arange(S)
    scale = dh ** -0.5

    def block(args):
        qi, i = args
        qpos = i * Q_BLOCK + jnp.arange(Q_BLOCK)
        causal = kpos[None, :] < qpos[:, None]
        z = jnp.einsum('bhqd,bhkd->bhqk', qi.astype(jnp.float32), kf) * scale
        log_stay = jnp.where(causal, jax.nn.log_sigmoid(-z), 0.0)
        after = lax.cumsum(log_stay, axis=3, reverse=True) - log_stay
        w = jnp.where(causal, jnp.exp(jax.nn.log_sigmoid(z) + after), 0.0)
        return jnp.einsum('bhqk,bhkd->bhqd', w.astype(v.dtype), v)

    out = lax.map(block, (qb, jnp.arange(nb)))
    return out.transpose(1, 2, 0, 3, 4).reshape(B, H, S, dh)


def spatial_gating(uv, g_v, w_s, b_s):
    B, S, _ = uv.shape
    u, v = jnp.split(jax.nn.gelu(uv), 2, axis=-1)
    vf = v.astype(jnp.float32)
    mu = jnp.mean(vf, axis=-1, keepdims=True)
    var = jnp.mean(jnp.square(vf - mu), axis=-1, keepdims=True)
    vn = ((vf - mu) * lax.rsqrt(var + EPS)).astype(v.dtype) * g_v
    w_causal = w_s * jnp.tril(jnp.ones((CHUNK, CHUNK), w_s.dtype))
    vc = vn.reshape(B, S // CHUNK, CHUNK, SGU_GROUPS, SGU_GROUP_DIM)
    mix = jnp.einsum('gts,bnsgc->bntgc', w_causal, vc) + b_s.T[None, None, :, :, None]
    return u * mix.reshape(B, S, SGU_WIDTH)


def hierarchical_moe(xn, w_rg, b_rg, w_re, b_re, w_gate, w_up, w_down):
    B, S, D = xn.shape
    N = B * S
    xt = xn.reshape(N, D)
    g_logits = (xt @ w_rg + b_rg).astype(jnp.float32)
    g_prob = jax.nn.softmax(g_logits, axis=-1)
    g_sel = jnp.argmax(g_logits, axis=-1)
    g_w = jnp.take_along_axis(g_prob, g_sel[:, None], axis=-1)[:, 0]
    e_logits = (xt @ w_re + b_re).astype(jnp.float32).reshape(N, N_GROUPS, EXPERTS_PER_GROUP)
    e_logits = jnp.take_along_axis(e_logits, g_sel[:, None, None], axis=1)[:, 0]
    top_l, top_j = lax.top_k(e_logits, TOP_K)
    top_w = jax.nn.softmax(top_l, axis=-1) * g_w[:, None]
    eid = (g_sel[:, None] * EXPERTS_PER_GROUP + top_j).reshape(-1)
    tok = jnp.repeat(jnp.arange(N), TOP_K)
    wts = top_w.reshape(-1)
    M = eid.shape[0]

    order = jnp.argsort(eid)
    e_s, tok_s, w_s = eid[order], tok[order], wts[order]
    counts = jnp.bincount(eid, length=N_EXPERTS)
    padded = (counts + EXPERT_BLOCK - 1) // EXPERT_BLOCK * EXPERT_BLOCK
    start = jnp.cumsum(counts) - counts
    pend = jnp.cumsum(padded)
    pstart = pend - padded
    dest = pstart[e_s] + jnp.arange(M) - start[e_s]
    m_pad = M + N_EXPERTS * EXPERT_BLOCK
    nblk = m_pad // EXPERT_BLOCK
    tok_buf = jnp.full((m_pad,), N, jnp.int32).at[dest].set(tok_s.astype(jnp.int32))
    w_buf = jnp.zeros((m_pad,), jnp.float32).at[dest].set(w_s)
    blk_e = jnp.minimum(jnp.searchsorted(pend, jnp.arange(nblk) * EXPERT_BLOCK, side='right'),
                        N_EXPERTS - 1)
    x_pad = jnp.concatenate([xt, jnp.zeros((1, D), xt.dtype)], axis=0)
    xb = x_pad[tok_buf].reshape(nblk, EXPERT_BLOCK, D)

    def expert_block(args):
        xblk, e = args
        hid = jax.nn.silu(xblk @ w_gate[e]) * (xblk @ w_up[e])
        return hid @ w_down[e]

    yb = lax.map(expert_block, (xb, blk_e)).reshape(m_pad, D)
    y = jax.ops.segment_sum(yb.astype(jnp.float32) * w_buf[:, None], tok_buf, num_segments=N + 1)[:N]
    return y.astype(xn.dtype).reshape(B, S, D)


def setup_inputs(seed: int = 0) -> dict:
    key = jax.random.key(seed)
    ks = jax.random.split(key, 24)
    f32 = jnp.float32
    L, D = DEPTH, D_MODEL
    nrm = lambda k, shp, s: jax.random.normal(k, shp, f32) * s
    return {
        "x": nrm(ks[0], (BATCH, SEQ, D), 1.0),
        "c": nrm(ks[1], (BATCH, D), 1.0),
        "g_mix": 1.0 + nrm(ks[2], (L, D), 0.02),
        "g_ffn": 1.0 + nrm(ks[3], (L, D), 0.02),
        "w_ada": nrm(ks[4], (L, D, N_MOD * D), 0.5 * D ** -0.5),
        "b_ada": nrm(ks[5], (L, N_MOD * D), 0.02),
        "w_in": nrm(ks[6], (L, D, IN_COLS), D ** -0.5),
        "w_sba_out": nrm(ks[7], (L, SBA_WIDTH, D), SBA_WIDTH ** -0.5),
        "g_sgu": 1.0 + nrm(ks[8], (L, SGU_WIDTH), 0.02),
        "w_spatial": nrm(ks[9], (L, SGU_GROUPS, CHUNK, CHUNK), CHUNK ** -0.5),
        "b_spatial": 1.0 + nrm(ks[10], (L, SGU_GROUPS, CHUNK), 0.02),
        "w_sgu_out": nrm(ks[11], (L, SGU_WIDTH, D), SGU_WIDTH ** -0.5),
        "w_out": nrm(ks[12], (L, D, D), D ** -0.5),
        "w_router_group": nrm(ks[13], (L, D, N_GROUPS), D ** -0.5),
        "b_router_group": nrm(ks[14], (L, N_GROUPS), 0.01),
        "w_router_expert": nrm(ks[15], (L, D, N_EXPERTS), D ** -0.5),
        "b_router_expert": nrm(ks[16], (L, N_EXPERTS), 0.01),
        "w_expert_gate": nrm(ks[17], (L, N_EXPERTS, D, D_EXPERT), D ** -0.5),
        "w_expert_up": nrm(ks[18], (L, N_EXPERTS, D, D_EXPERT), D ** -0.5),
        "w_expert_down": nrm(ks[19], (L, N_EXPERTS, D_EXPERT, D), D_EXPERT ** -0.5),
        "g_final": 1.0 + nrm(ks[20], (D,), 0.02),
    }


def reference(x, c, g_mix, g_ffn, w_ada, b_ada, w_in, w_sba_out, g_sgu, w_spatial, b_spatial,
              w_sgu_out, w_out, w_router_group, b_router_group, w_router_expert, b_router_expert,
              w_expert_gate, w_expert_up, w_expert_down, g_final):
    B, S, D = x.shape
    h = x
    c_act = jax.nn.silu(c)
    splits = [SBA_WIDTH, 2 * SBA_WIDTH, 3 * SBA_WIDTH, 3 * SBA_WIDTH + 2 * SGU_WIDTH]

    def heads(t):
        return t.reshape(B, S, SBA_HEADS, SBA_HEAD_DIM).transpose(0, 2, 1, 3)

    for l in range(DEPTH):
        mod = c_act @ w_ada[l] + b_ada[l]
        sh_m, sc_m, gt_m, sh_f, sc_f, gt_f = jnp.split(mod, N_MOD, axis=-1)

        n = modulate(rms_norm(h, g_mix[l]), sh_m, sc_m)
        proj = n @ w_in[l]
        q, k, v, uv, gates = jnp.split(proj, splits, axis=-1)
        y_a = stick_breaking_attention(heads(q), heads(k), heads(v))
        y_a = y_a.transpose(0, 2, 1, 3).reshape(B, S, SBA_WIDTH)
        y_b = spatial_gating(uv, g_sgu[l], w_spatial[l], b_spatial[l])
        gate_a, gate_b = jnp.split(jax.nn.sigmoid(gates), N_BRANCHES, axis=-1)
        merged = gate_a * (y_a @ w_sba_out[l]) + gate_b * (y_b @ w_sgu_out[l])
        h = h + gt_m[:, None, :] * (merged @ w_out[l])

        n = modulate(rms_norm(h, g_ffn[l]), sh_f, sc_f)
        y = hierarchical_moe(n, w_router_group[l], b_router_group[l], w_router_expert[l],
                             b_router_expert[l], w_expert_gate[l], w_expert_up[l], w_expert_down[l])
        h = h + gt_f[:, None, :] * y
    return rms_norm(h, g_final)
```

```python
import numpy as np
from contextlib import ExitStack
import concourse.bass as bass
import concourse.mybir as mybir
from concourse.bass_utils import run_bass_kernel_spmd

F32 = mybir.dt.float32
BF16 = mybir.dt.bfloat16
I32 = mybir.dt.int32
AF = mybir.ActivationFunctionType
ALU = mybir.AluOpType
AX = mybir.AxisListType
ET = mybir.EngineType

S = 8192
D = 1024
NT = S // 128
GT = 2
GW = GT * 128
NG = NT // GT
ATT_W = 1
NKEY = (1 + ATT_W) * 128
NSLOT = GT + ATT_W
EPS = 1e-6
EPAD = 256
NBLK = 192
MPAD = NBLK * 128
C_Q, C_K, C_V, C_U, C_VS, C_GA, C_GB = 0, 512, 1024, 1536, 2048, 2560, 3584

SAME_ENGINE_SYNC = True
RAW_ONLY = True


class Buf:
    def __init__(self, name):
        self.name = name
        self.w = {}
        self.r = {}


class Sched:
    ENGS = ("pe", "act", "dve", "pool", "sp")

    def __init__(self, nc, es):
        self.nc = nc
        self.es = es
        self.ops = {e: [] for e in self.ENGS}
        self.sems = {}
        self.cnt = {}
        self.known = {e: {} for e in self.ENGS}
        for e in self.ENGS:
            self._sem("E_" + e)

    def _sem(self, key):
        if key not in self.sems:
            self.sems[key] = self.es.enter_context(self.nc.semaphore(key))
            self.cnt[key] = 0
        return key

    def _deps(self, eng, reads, writes, own_key):
        waits = {}

        def need(k, v, raw):
            if k == own_key:
                if eng == "pe" or not SAME_ENGINE_SYNC or (RAW_ONLY and not raw):
                    return
            if self.known[eng].get(k, 0) >= v:
                return
            waits[k] = max(waits.get(k, 0), v)

        for b in reads:
            for k, v in b.w.items():
                need(k, v, True)
        for b in writes:
            for k, v in b.w.items():
                need(k, v, False)
            for k, v in b.r.items():
                need(k, v, False)
        for k, v in waits.items():
            self.known[eng][k] = v
        return list(waits.items())

    def _commit(self, key, inc, reads, writes):
        self.cnt[key] += inc
        v = self.cnt[key]
        for b in reads:
            b.r[key] = max(b.r.get(key, 0), v)
        for b in writes:
            b.w[key] = max(b.w.get(key, 0), v)

    def op(self, eng, fn, reads=(), writes=()):
        key = "E_" + eng
        waits = self._deps(eng, reads, writes, key)
        self.ops[eng].append((waits, fn, key, 1))
        self._commit(key, 1, reads, writes)

    def dma(self, eng, fn, reads=(), writes=(), key="dma"):
        key = self._sem("D_" + key + ("_sw" if eng == "pool" else ""))
        waits = self._deps(eng, reads, writes, None)
        self.ops[eng].append((waits, fn, key, 16))
        self._commit(key, 16, reads, writes)

    def barrier(self, exclude=()):
        snap = {k: v for k, v in self.cnt.items() if k not in exclude}
        for e in self.ENGS:
            waits = []
            for k, v in snap.items():
                if v > 0 and self.known[e].get(k, 0) < v and k != "E_" + e:
                    waits.append((k, v))
                    self.known[e][k] = v
            if waits:
                self.ops[e].append((waits, None, None, 0))

    def _simulate(self):
        st = getattr(self, "_simstate", None)
        if st is None:
            st = self._simstate = {}
        pc = {e: 0 for e in self.ENGS}
        progress = True
        while progress:
            progress = False
            for e in self.ENGS:
                ops = self.ops[e]
                while pc[e] < len(ops):
                    waits, fn, key, inc = ops[pc[e]]
                    if any(st.get(k, 0) < v for k, v in waits):
                        break
                    if key is not None:
                        st[key] = st.get(key, 0) + inc
                    pc[e] += 1
                    progress = True
        stuck = {e: (pc[e], len(self.ops[e])) for e in self.ENGS if pc[e] < len(self.ops[e])}
        if stuck:
            msg = []
            for e, (p, n) in stuck.items():
                waits, fn, key, inc = self.ops[e][p]
                msg.append(f"{e}@{p}/{n} waits {[(k, v, st.get(k, 0)) for k, v in waits if st.get(k, 0) < v]} line {fn.__code__.co_firstlineno if fn else None}")
            raise RuntimeError("DEADLOCK in schedule: " + " | ".join(msg))

    def emit(self, final=False):
        self._simulate()
        with self.nc.Block() as block:
            def run(eng_name):
                ops = self.ops[eng_name]

                def body(eng):
                    for waits, fn, key, inc in ops:
                        for k, v in waits:
                            eng.wait_ge(self.sems[k], v)
                        if fn is not None:
                            ins = fn(eng)
                            ins.then_inc(self.sems[key], inc)
                    if final:
                        for k, v in self.cnt.items():
                            if v > 0 and k != "E_" + eng_name:
                                eng.wait_ge(self.sems[k], v)
                return body
            block.tensor(run("pe"))
            block.scalar(run("act"))
            block.vector(run("dve"))
            block.gpsimd(run("pool"))
            block.sync(run("sp"))
        self.ops = {e: [] for e in self.ENGS}


def sb_bcast(ap, shape_steps):
    return bass.AP(tensor=ap.tensor, offset=ap.offset, ap=[list(ap.ap[0])] + [list(s) for s in shape_steps])


def build_program(debug=False, n_groups=NG, do_moe=True, stop=None):
    nc = bass.Bass("TRN2", target_bir_lowering=False)
    dt_ = nc.dram_tensor
    kin = "ExternalInput"
    x_d = dt_("x", [S, D], F32, kind=kin)
    c_d = dt_("c2", [128, 8], F32, kind=kin)
    gmix_d = dt_("gmix2", [128, 8], F32, kind=kin)
    gffn_d = dt_("g_ffn", [1, D], F32, kind=kin)
    gfin_d = dt_("g_final", [1, D], F32, kind=kin)
    wada_d = dt_("w_ada", [D, 6 * D], F32, kind=kin)
    bada_d = dt_("b_ada", [1, 6 * D], F32, kind=kin)
    win_d = dt_("w_in", [D, 4608], F32, kind=kin)
    wsba_d = dt_("w_sba_out", [512, D], F32, kind=kin)
    gsgu_d = dt_("g_sgu", [1, 512], F32, kind=kin)
    wsp_d = dt_("w_spatial", [8, 128, 128], F32, kind=kin)
    bsp_d = dt_("b_spatial", [8, 128], F32, kind=kin)
    wsgu_d = dt_("w_sgu_out", [512, D], F32, kind=kin)
    wout_d = dt_("w_out", [D, D], F32, kind=kin)
    wr_d = dt_("w_r", [D, 36], F32, kind=kin)
    br_d = dt_("b_r", [1, 36], F32, kind=kin)
    weg_d = dt_("w_eg", [32, D, 512], F32, kind=kin)
    weu_d = dt_("w_eu", [32, D, 512], F32, kind=kin)
    wed_d = dt_("w_ed", [32, 512, D], F32, kind=kin)
    out_d = dt_("out", [S, D], F32, kind="ExternalOutput")
    dk = "ExternalOutput" if debug else "Internal"
    mod_d = dt_("mod_d", [1, 6 * D], F32, kind="Internal")
    h1_d = dt_("h1_d", [S, D], F32, kind=dk)
    n2_d = dt_("n2_d", [S + 1, D], BF16, kind="Internal")
    yb_d = dt_("yb_d", [MPAD, D], F32, kind="Internal")
    tok_d = dt_("tok_d", [MPAD, 1], I32, kind=dk)
    wegb_d = dt_("wegb_d", [32 * 128, 4096], BF16, kind="Internal")
    weub_d = dt_("weub_d", [32 * 128, 4096], BF16, kind="Internal")
    wedb_d = dt_("wedb_d", [32 * 128, 4096], BF16, kind="Internal")
    if debug:
        dbg_logits_d = dt_("dbg_logits", [128, NT * 36], F32, kind="ExternalOutput")
        dbg_dest_d = dt_("dbg_dest", [128, NT * 2], F32, kind="ExternalOutput")
        dbg_tw_d = dt_("dbg_tw", [128, NT * 2], F32, kind="ExternalOutput")
        dbg_blk_d = dt_("dbg_blk", [1, NBLK], I32, kind="ExternalOutput")

    ident_c = nc.inline_tensor(np.eye(128, dtype=np.float32), "ident_c")
    J_c = nc.inline_tensor(np.ascontiguousarray(np.eye(128, dtype=np.float32)[::-1]), "J_c")
    p_i = np.arange(128)[:, None]
    j_i = np.arange(NKEY)[None, :]
    mA = np.zeros((128, NKEY), np.float32)
    mA[:, :128] = (j_i[:, :128] <= 127 - p_i)
    mB = mA.copy()
    mB[:, 128:] = 1.0
    maskA_c = nc.inline_tensor(mA, "maskA_c")
    maskB_c = nc.inline_tensor(mB, "maskB_c")
    tril_c = nc.inline_tensor(np.tril(np.ones((128, 128), np.float32)), "tril_c")
    tris_c = nc.inline_tensor(np.triu(np.ones((32, 32), np.float32), 1), "tris_c")
    tokid_c = nc.inline_tensor((np.arange(NT)[None, :] * 128 + np.arange(128)[:, None]).astype(np.int32), "tokid_c")
    pidx_c = nc.inline_tensor(np.broadcast_to(np.arange(128, dtype=np.int32)[:, None], (128, NBLK)).copy(), "pidx_c")
    jrow_c = nc.inline_tensor(np.broadcast_to((np.arange(NBLK) * 128.0).astype(np.float32), (32, NBLK)).copy(), "jrow_c")

    es = ExitStack()
    with es:
        sc = Sched(nc, es)
        E = es.enter_context

        def sbt(name, shape, dtype):
            return E(nc.sbuf_tensor(name, shape, dtype))

        def pst(name, shape, dtype):
            return E(nc.psum_tensor(name, shape, dtype))

        ident_f = sbt("ident_f", [128, 128], F32)
        ident_b = sbt("ident_b", [128, 128], BF16)
        J_b = sbt("J_b", [128, 128], BF16)
        logits = sbt("logits", [128, NT, 36], F32)
        B_const = Buf("const")
        B_h1d = Buf("h1_d")
        B_n2d = Buf("n2_d")
        B_logits = Buf("logits")
        B_rows = Buf("rows")

        sc.dma("sp", lambda e: e.dma_start(out=ident_f[:], in_=ident_c.ap()), writes=[B_const], key="c_ident")
        J_f = sbt("J_f", [128, 128], F32)
        sc.dma("sp", lambda e: e.dma_start(out=J_f[:], in_=J_c.ap()), writes=[B_const], key="c_J")
        sc.op("dve", lambda e: e.tensor_copy(out=ident_b[:], in_=ident_f[:]), reads=[B_const], writes=[B_const])
        sc.op("dve", lambda e: e.tensor_copy(out=J_b[:], in_=J_f[:]), reads=[B_const], writes=[B_const])

        p1 = ExitStack()
        with p1:
            P = p1.enter_context

            def sb1(name, shape, dtype):
                return P(nc.sbuf_tensor(name, shape, dtype))

            def ps1(name, shape, dtype):
                return P(nc.psum_tensor(name, shape, dtype))

            win_sb = sb1("win_sb", [128, 8, 4608], BF16)
            wsba_sb = sb1("wsba_sb", [128, 4, D], BF16)
            wsgu_sb = sb1("wsgu_sb", [128, 4, D], BF16)
            wout_sb = sb1("wout_sb", [128, 8, D], BF16)
            wr_hi = sb1("wr_hi", [128, 8, 36], BF16)
            wr_lo = sb1("wr_lo", [128, 8, 36], BF16)
            br_row = sb1("br_row", [128, 36], F32)
            wct_sb = sb1("wct_sb", [128, 8, 128], BF16)
            bsb = sb1("bsb", [128, 4, 128], F32)
            gsgu_row = sb1("gsgu_row", [128, 512], F32)
            af_row = sb1("af_row", [128, D], F32)
            shf_row = sb1("shf_row", [128, D], F32)
            am_col = sb1("am_col", [128, 8], F32)
            shm_col = sb1("shm_col", [128, 8], F32)
            maskA = sb1("maskA", [128, NKEY], F32)
            B_w = Buf("weights")

            psP = [ps1(f"psP{i}", [128, 512], F32) for i in range(4)]
            psY = ps1("psY", [128, 512], F32)
            psTW = [ps1(f"psTW{i}", [128, 512], BF16) for i in range(2)]
            psT = [psTW[0][:, 0:GW], psTW[1][:, 0:GW]]
            psW = [psTW[0][:, GW:GW + NKEY], psTW[1][:, GW:GW + NKEY]]
            psZ_all = ps1("psZ_all", [128, 2 * NKEY], F32)
            psZ = [psZ_all[:, 0:NKEY], psZ_all[:, NKEY:2 * NKEY]]
            B_psP = [Buf(f"psP{i}") for i in range(4)]
            B_psY = Buf("psY")
            B_psT = [Buf(f"psT{i}") for i in range(2)]
            B_psZ = [Buf(f"psZ{i}") for i in range(2)]
            B_psW = [Buf(f"psW{i}") for i in range(2)]
            pp = [0]

            def next_psP():
                i = pp[0] % 4
                pp[0] += 1
                return psP[i], B_psP[i]

            for k in range(8):
                sc.dma("pool", lambda e, k=k: e.dma_start(out=win_sb[:, k, :], in_=win_d.ap()[k * 128:(k + 1) * 128, :]),
                       writes=[B_w], key="setup")
            for k in range(4):
                sc.dma("pool", lambda e, k=k: e.dma_start(out=wsba_sb[:, k, :], in_=wsba_d.ap()[k * 128:(k + 1) * 128, :]),
                       writes=[B_w], key="setup")
                sc.dma("pool", lambda e, k=k: e.dma_start(out=wsgu_sb[:, k, :], in_=wsgu_d.ap()[k * 128:(k + 1) * 128, :]),
                       writes=[B_w], key="setup")
            sc.dma("sp", lambda e: e.dma_start(out=br_row[:], in_=bass.AP(tensor=br_d.ap().tensor, offset=0, ap=[[0, 128], [1, 36]])),
                   writes=[B_w], key="setup")
            sc.dma("sp", lambda e: e.dma_start(out=gsgu_row[:], in_=bass.AP(tensor=gsgu_d.ap().tensor, offset=0, ap=[[0, 128], [1, 512]])),
                   writes=[B_w], key="setup")
            sc.dma("sp", lambda e: e.dma_start(out=maskA[:], in_=maskA_c.ap()), writes=[B_w], key="setup")
            for g in range(8):
                sc.dma("sp", lambda e, g=g: e.dma_start(
                    out=bsb[(g % 2) * 64:(g % 2) * 64 + 64, g // 2, :],
                    in_=bass.AP(tensor=bsp_d.ap().tensor, offset=g * 128, ap=[[0, 64], [1, 128]])),
                    writes=[B_w], key="setup")

            B_wexp = Buf("wexp")
            if do_moe:
                for e_ in range(32):
                    for src, dst in ((weg_d, wegb_d), (weu_d, weub_d), (wed_d, wedb_d)):
                        sc.dma("pool", lambda e, src=src, dst=dst, e_=e_: e.dma_start(
                            out=dst.ap()[e_ * 128:(e_ + 1) * 128, :], in_=src.ap()[e_].rearrange("(p k) n -> p (k n)", p=128)),
                            writes=[B_wexp], key="wconv")

            p0 = ExitStack()
            with p0:
                P0 = p0.enter_context
                c_sb = P0(nc.sbuf_tensor("c_sb", [128, 8], F32))
                cact = P0(nc.sbuf_tensor("cact", [128, 8], F32))
                wada_sb = [P0(nc.sbuf_tensor(f"wada_sb{i}", [128, 8, 256], F32)) for i in range(2)]
                bada_sb = [P0(nc.sbuf_tensor(f"bada_sb{i}", [1, 256], F32)) for i in range(2)]
                mod_sb = [P0(nc.sbuf_tensor(f"mod_sb{i}", [1, 256], F32)) for i in range(2)]
                modc = P0(nc.sbuf_tensor("modc", [128, 48], F32))
                gmix_sb = P0(nc.sbuf_tensor("gmix_sb", [128, 8], F32))
                gtm_row = P0(nc.sbuf_tensor("gtm_row", [128, D], F32))
                gffn_row = P0(nc.sbuf_tensor("gffn_row", [128, D], F32))
                scf_row = P0(nc.sbuf_tensor("scf_row", [128, D], F32))
                wout_f = [P0(nc.sbuf_tensor(f"wout_f{i}", [128, D], F32)) for i in range(2)]
                wsp_f = P0(nc.sbuf_tensor("wsp_f", [128, 8, 128], F32))
                wsp_b = P0(nc.sbuf_tensor("wsp_b", [128, 8, 128], BF16))
                tril_sb = P0(nc.sbuf_tensor("tril_sb", [128, 128], F32))
                zrow = P0(nc.sbuf_tensor("zrow", [1, D], BF16))
                wr_sb = P0(nc.sbuf_tensor("wr_sb", [128, 8, 36], F32))
                wr_t = P0(nc.sbuf_tensor("wr_t", [128, 8, 36], F32))
                B_wr = Buf("wr_sb")
                sc.dma("sp", lambda e: e.dma_start(out=wr_sb[:], in_=wr_d.ap().rearrange("(k p) n -> p k n", p=128)),
                       writes=[B_wr], key="c_wr")
                sc.op("dve", lambda e: e.tensor_copy(out=wr_hi[:], in_=wr_sb[:]), reads=[B_wr], writes=[B_w])
                sc.op("dve", lambda e: e.tensor_tensor(out=wr_t[:], in0=wr_sb[:], in1=wr_hi[:], op=ALU.subtract), reads=[B_wr, B_w], writes=[B_wr])
                sc.op("dve", lambda e: e.tensor_copy(out=wr_lo[:], in_=wr_t[:]), reads=[B_wr], writes=[B_w])
                B_c = Buf("c")
                B_wada = [Buf("wada0"), Buf("wada1")]
                B_bada = [Buf("bada0"), Buf("bada1")]
                B_mod = [Buf("mod_sb0"), Buf("mod_sb1")]
                B_modd = Buf("mod_d")
                B_modc = Buf("modc")
                B_p0 = Buf("p0misc")
                B_woutf = [Buf("woutf0"), Buf("woutf1")]

                sc.op("dve", lambda e: e.memset(zrow[:], 0.0), writes=[B_p0])
                sc.dma("sp", lambda e: e.dma_start(out=n2_d.ap()[S:S + 1, :], in_=zrow[:]), reads=[B_p0], writes=[B_n2d], key="zrow")
                sc.dma("sp", lambda e: e.dma_start(out=c_sb[:], in_=c_d.ap()), writes=[B_c], key="p0a_1")
                sc.dma("sp", lambda e: e.dma_start(out=gmix_sb[:], in_=gmix_d.ap()), writes=[B_c], key="p0a_2")
                sc.dma("sp", lambda e: e.dma_start(out=tril_sb[:], in_=tril_c.ap()), writes=[B_p0], key="p0a_3")
                sc.dma("sp", lambda e: e.dma_start(out=wsp_f[:], in_=wsp_d.ap().rearrange("g t s -> t g s")),
                       writes=[B_p0], key="p0a_4")
                sc.dma("sp", lambda e: e.dma_start(out=gffn_row[:], in_=bass.AP(tensor=gffn_d.ap().tensor, offset=0, ap=[[0, 128], [1, D]])),
                       writes=[B_p0], key="p0a_5")
                sc.op("act", lambda e: e.activation(out=cact[:], in_=c_sb[:], func=AF.Silu), reads=[B_c], writes=[B_c])
                for n in range(24):
                    sl = n % 2
                    sc.dma("sp", lambda e, n=n, sl=sl: e.dma_start(
                        out=wada_sb[sl][:], in_=wada_d.ap()[:, n * 256:(n + 1) * 256].rearrange("(k p) n -> p k n", p=128)),
                        writes=[B_wada[sl]], key=f"wada{sl}")
                    sc.dma("sp", lambda e, n=n, sl=sl: e.dma_start(out=bada_sb[sl][:], in_=bada_d.ap()[:, n * 256:(n + 1) * 256]),
                           writes=[B_bada[sl]], key=f"bada{sl}")
                    pt, bpt = next_psP()
                    for k in range(8):
                        sc.op("pe", lambda e, pt=pt, sl=sl, k=k: e.matmul(pt[0:1, 0:256], lhsT=cact[:, k:k + 1], rhs=wada_sb[sl][:, k, :],
                                                                        start=(k == 0), stop=(k == 7)),
                              reads=[B_c, B_wada[sl]], writes=[bpt])
                    sc.op("dve", lambda e, pt=pt, sl=sl: e.tensor_tensor(out=mod_sb[sl][:], in0=pt[0:1, 0:256], in1=bada_sb[sl][:], op=ALU.add),
                          reads=[bpt, B_bada[sl]], writes=[B_mod[sl]])
                    sc.dma("sp", lambda e, n=n, sl=sl: e.dma_start(out=mod_d.ap()[:, n * 256:(n + 1) * 256], in_=mod_sb[sl][:]),
                           reads=[B_mod[sl]], writes=[B_modd], key=f"p0b{sl}")
                sc.dma("sp", lambda e: e.dma_start(out=modc[:], in_=mod_d.ap().rearrange("o (j p) -> p (o j)", p=128),
                                                   allow_slow_non_contiguous=True),
                       reads=[B_modd], writes=[B_modc], key="p0c_6")

                def rowb(off):
                    return bass.AP(tensor=mod_d.ap().tensor, offset=off, ap=[[0, 128], [1, D]])
                sc.dma("sp", lambda e: e.dma_start(out=gtm_row[:], in_=rowb(2 * D)), reads=[B_modd], writes=[B_modc], key="p0c_7")
                sc.dma("sp", lambda e: e.dma_start(out=shf_row[:], in_=rowb(3 * D)), reads=[B_modd], writes=[B_modc], key="p0c_8")
                sc.dma("sp", lambda e: e.dma_start(out=scf_row[:], in_=rowb(4 * D)), reads=[B_modd], writes=[B_modc], key="p0c_9")
                sc.op("dve", lambda e: e.scalar_tensor_tensor(out=am_col[:], in0=modc[:, 8:16], scalar=1.0, in1=gmix_sb[:],
                                                              op0=ALU.add, op1=ALU.mult),
                      reads=[B_modc, B_c], writes=[B_w])
                sc.op("dve", lambda e: e.tensor_copy(out=shm_col[:], in_=modc[:, 0:8]), reads=[B_modc], writes=[B_w])
                sc.op("dve", lambda e: e.scalar_tensor_tensor(out=af_row[:], in0=scf_row[:], scalar=1.0, in1=gffn_row[:],
                                                              op0=ALU.add, op1=ALU.mult),
                      reads=[B_modc, B_p0], writes=[B_w])
                for k in range(8):
                    sl = k % 2
                    sc.dma("sp", lambda e, k=k, sl=sl: e.dma_start(out=wout_f[sl][:], in_=wout_d.ap()[k * 128:(k + 1) * 128, :]),
                           writes=[B_woutf[sl]], key=f"woutf{sl}")
                    sc.op("dve", lambda e, k=k, sl=sl: e.tensor_tensor(out=wout_sb[:, k, :], in0=wout_f[sl][:], in1=gtm_row[:], op=ALU.mult),
                          reads=[B_woutf[sl], B_modc], writes=[B_w])
                for g in range(8):
                    sc.op("dve", lambda e, g=g: e.tensor_tensor(out=wsp_b[:, g, :], in0=wsp_f[:, g, :], in1=tril_sb[:], op=ALU.mult),
                          reads=[B_p0], writes=[B_p0])
                for g in range(8):
                    h = g % 2
                    sc.op("pe", lambda e, g=g, h=h: e.transpose(psT[h][:, 0:128], wsp_b[:, g, :], ident_b[:]),
                          reads=[B_p0, B_const], writes=[B_psT[h]])
                    sc.op("act", lambda e, g=g, h=h: e.activation(out=wct_sb[:, g, :], in_=psT[h][:, 0:128], func=AF.Copy),
                          reads=[B_psT[h]], writes=[B_w])
                sc.barrier(exclude=("D_wconv_sw",))
                sc.emit(final=(stop == 'p0'))
                if stop == 'p0':
                    return nc

            xg2 = [sb1(f"xg{i}", [128, GT, D], F32) for i in range(2)]
            B_xg2 = [Buf("xg0"), Buf("xg1")]
            xs = sb1("xs", [128, GT, D], BF16)
            B_xs = Buf("xs")
            stat = sb1("stat", [128, 32], F32)
            B_ss = Buf("ss")
            B_rstd = Buf("rstd")
            nT = sb1("nT", [128, 8, GW], BF16)
            B_nT = Buf("nT")
            qT = sb1("qT", [128, 4, GW], BF16)
            B_qT = Buf("qT")
            uT = sb1("uT", [128, 4, GW], F32)
            B_uT = Buf("uT")
            k_tm = sb1("k_tm", [128, GT, 512], BF16)
            v_tm = sb1("v_tm", [128, GT, 512], BF16)
            vs = sb1("vs", [128, GT, 512], F32)
            B_ktm = Buf("k_tm")
            B_vtm = Buf("v_tm")
            B_vs = Buf("vs")
            kTr = sb1("kTr", [128, 4, NSLOT * 128], BF16)
            vr = sb1("vr", [128, NSLOT, 512], BF16)
            B_kTr = Buf("kTr")
            B_vr = Buf("vr")
            r_sb = [sb1(f"r_sb{i}", [128, NKEY], F32) for i in range(2)]
            Pb = [sb1(f"Pb{i}", [128, NKEY + 1], F32) for i in range(2)]
            w_bf = [sb1(f"w_bf{i}", [128, NKEY], BF16) for i in range(2)]
            wT = [sb1(f"wT{i}", [128, NKEY], BF16) for i in range(2)]
            B_r = [Buf("r0"), Buf("r1")]
            B_Pb = [Buf("Pb0"), Buf("Pb1")]
            B_wbf = [Buf("wbf0"), Buf("wbf1")]
            B_wT = [Buf("wT0"), Buf("wT1")]
            yaT = sb1("yaT", [128, 4, GW], BF16)
            ybT = sb1("ybT", [128, 4, GW], BF16)
            B_yaT = Buf("yaT")
            B_ybT = Buf("ybT")
            vn = sb1("vn", [128, 512], F32)
            vnb2 = sb1("vnb2", [128, GT, 512], BF16)
            B_vn = Buf("vn")
            B_vnb = Buf("vnb")
            bnst = sb1("bnst", [128, GT, 6], F32)
            mv = sb1("mv", [128, GT, 2], F32)
            B_mv = Buf("mv")
            tmpf = [sb1("tmpf0", [128, 512], F32), sb1("tmpf1", [128, GW], F32), sb1("tmpf2", [128, GW], F32)]
            B_tmpf = [Buf(f"tmpf{i}") for i in range(3)]
            maskB = tmpf[1]
            sc.dma("sp", lambda e: e.dma_start(out=maskB[:], in_=maskB_c.ap()), writes=[B_tmpf[1]], key="c_maskB")
            mT = sb1("mT", [128, 8, GW], BF16)
            B_mT = Buf("mT")
            n2f = sb1("n2f", [128, D], F32)
            B_n2f = Buf("n2f")
            n2b = sb1("n2b", [128, D], BF16)
            B_n2b = Buf("n2b")
            junk = n2b
            B_junk = B_n2b
            hiT = sb1("hiT", [128, 8, 128], BF16)
            loT = sb1("loT", [128, 8, 128], BF16)
            B_n2T = Buf("n2T")
            n2lo = tmpf[0][:].bitcast(BF16)

            for i in range(2):
                sc.op("dve", lambda e, i=i: e.memset(Pb[i][:, 0:1], 1.0), writes=[B_Pb[i]])
            sc.op("dve", lambda e: e.memset(kTr[:], 0.0), writes=[B_kTr])
            sc.op("dve", lambda e: e.memset(vr[:], 0.0), writes=[B_vr])

            def grp_ctx(G):
                return xg2[G % 2], B_xg2[G % 2]

            def g_load(G):
                xg, B_xg = grp_ctx(G)
                sc.dma("sp", lambda e, G=G: e.dma_start(out=xg[:], in_=x_d.ap()[G * GW:(G + 1) * GW, :].rearrange("(i p) d -> p i d", p=128)),
                       writes=[B_xg], key=f"xg{G % 2}")

            def g_front_a(G):
                X, BX = grp_ctx(G)
                sc.op("dve", lambda e: e.memset(stat[:, 0:4], 0.0), writes=[B_ss])
                for i in range(GT):
                    sc.op("act", lambda e, i=i: e.activation(out=junk[:], in_=X[:, i, :], func=AF.Square, accum_out=stat[:, i:i + 1]),
                          reads=[BX, B_ss], writes=[B_junk, B_ss])
                sc.op("act", lambda e: e.activation(out=stat[:, 4:4 + GT], in_=stat[:, 0:GT], func=AF.Sqrt, scale=1.0 / D, bias=EPS),
                      reads=[B_ss], writes=[B_ss])
                sc.op("dve", lambda e: e.reciprocal(out=stat[:, 8:8 + GT], in_=stat[:, 4:4 + GT]), reads=[B_ss], writes=[B_rstd])
                for i in range(GT):
                    sc.op("dve", lambda e, i=i: e.tensor_scalar(out=xs[:, i, :], in0=X[:, i, :], scalar1=stat[:, 8 + i:9 + i], scalar2=None,
                                                              op0=ALU.mult),
                          reads=[BX, B_rstd], writes=[B_xs])

            def g_front_b(G):
                X, BX = grp_ctx(G)
                for k in range(8):
                    tb = k % 2
                    for i in range(GT):
                        sc.op("pe", lambda e, k=k, i=i, tb=tb: e.transpose(psT[tb][:, i * 128:(i + 1) * 128], xs[:, i, k * 128:(k + 1) * 128], ident_b[:]),
                              reads=[B_xs, B_const], writes=[B_psT[tb]])
                    sc.op("act", lambda e, k=k, tb=tb: e.activation(out=nT[:, k, :], in_=psT[tb][:, 0:GW], func=AF.Identity,
                                                                  scale=am_col[:, k:k + 1], bias=shm_col[:, k:k + 1]),
                          reads=[B_psT[tb], B_w], writes=[B_nT])
                for m in range(4):
                    pt, bpt = next_psP()
                    for k in range(8):
                        sc.op("pe", lambda e, pt=pt, m=m, k=k: e.matmul(pt[:, 0:GW], lhsT=win_sb[:, k, C_Q + m * 128:C_Q + (m + 1) * 128], rhs=nT[:, k, :],
                                                                      start=(k == 0), stop=(k == 7)),
                              reads=[B_w, B_nT], writes=[bpt])
                    sc.op("act", lambda e, pt=pt, m=m: e.activation(out=qT[:, m, :], in_=pt[:, 0:GW], func=AF.Copy, scale=0.125),
                          reads=[bpt], writes=[B_qT])
                for i in range(GT):
                    for (col, dst, bdst) in ((C_K, k_tm, B_ktm), (C_V, v_tm, B_vtm)):
                        pt, bpt = next_psP()
                        for k in range(8):
                            sc.op("pe", lambda e, pt=pt, i=i, k=k, col=col: e.matmul(pt[:], lhsT=nT[:, k, i * 128:(i + 1) * 128],
                                                                                   rhs=win_sb[:, k, col:col + 512], start=(k == 0), stop=(k == 7)),
                                  reads=[B_w, B_nT], writes=[bpt])
                        sc.op("dve", lambda e, pt=pt, i=i, dst=dst: e.tensor_copy(out=dst[:, i, :], in_=pt[:]),
                              reads=[bpt], writes=[bdst])
                for i in range(GT):
                    pos = (GT - 1 - i)
                    pt, bpt = next_psP()
                    for c in range(4):
                        sc.op("pe", lambda e, pt=pt, i=i, c=c: e.matmul(pt[:, c * 128:(c + 1) * 128], lhsT=k_tm[:, i, c * 128:(c + 1) * 128], rhs=J_b[:],
                                                                      start=True, stop=True),
                              reads=[B_ktm, B_const], writes=[bpt])
                    sc.op("act", lambda e, pt=pt, pos=pos: e.activation(out=kTr[:, :, pos * 128:(pos + 1) * 128],
                                                                      in_=pt[:].rearrange("p (a b) -> p a b", a=4), func=AF.Copy),
                          reads=[bpt], writes=[B_kTr])
                    pt2, bpt2 = next_psP()
                    sc.op("pe", lambda e, pt2=pt2, i=i: e.matmul(pt2[:], lhsT=J_b[:], rhs=v_tm[:, i, :], start=True, stop=True),
                          reads=[B_vtm, B_const], writes=[bpt2])
                    sc.op("dve", lambda e, pt2=pt2, pos=pos: e.tensor_copy(out=vr[:, pos, :], in_=pt2[:]),
                          reads=[bpt2], writes=[B_vr])

                for i in range(GT):
                    pt, bpt = next_psP()
                    for k in range(8):
                        sc.op("pe", lambda e, pt=pt, i=i, k=k: e.matmul(pt[:], lhsT=nT[:, k, i * 128:(i + 1) * 128],
                                                                      rhs=win_sb[:, k, C_VS:C_VS + 512], start=(k == 0), stop=(k == 7)),
                              reads=[B_w, B_nT], writes=[bpt])
                    sc.op("act", lambda e, pt=pt, i=i: e.activation(out=vs[:, i, :], in_=pt[:], func=AF.Gelu_apprx_tanh),
                          reads=[bpt], writes=[B_vs])

            def g_back1a(G):
                X, BX = grp_ctx(G)
                for i in range(GT):
                    sc.op("dve", lambda e, i=i: e.bn_stats(out=bnst[:, i, :], in_=vs[:, i, :]), reads=[B_vs], writes=[B_mv])
                    sc.op("dve", lambda e, i=i: e.bn_aggr(out=mv[:, i, :], in_=bnst[:, i, :]), reads=[B_mv], writes=[B_mv])
                sc.op("act", lambda e: e.activation(out=stat[:, 12:12 + GT], in_=mv[:, :, 1], func=AF.Sqrt, scale=1.0, bias=EPS),
                      reads=[B_mv], writes=[B_ss])
                sc.op("dve", lambda e: e.reciprocal(out=stat[:, 16:16 + GT], in_=stat[:, 12:12 + GT]), reads=[B_ss], writes=[B_rstd])
                for i in range(GT):
                    sc.op("dve", lambda e, i=i: e.tensor_scalar(out=vn[:], in0=vs[:, i, :], scalar1=mv[:, i, 0:1], scalar2=stat[:, 16 + i:17 + i],
                                                              op0=ALU.subtract, op1=ALU.mult),
                          reads=[B_vs, B_mv, B_rstd], writes=[B_vn])
                    sc.op("dve", lambda e, i=i: e.tensor_tensor(out=vnb2[:, i, :], in0=vn[:], in1=gsgu_row[:], op=ALU.mult),
                          reads=[B_vn, B_w], writes=[B_vnb])
                hts = [(i, h) for i in range(GT) for h in (0, 2, 4, 6, 1, 3, 5, 7)]

                def att_A(n):
                    i, h = hts[n]
                    pos = GT - 1 - i
                    msk = maskB if (G == 0 and i == 0) else maskA
                    c = h // 2
                    ro = (h % 2) * 64
                    ab = n % 2
                    sc.op("pe", lambda e, ab=ab, c=c, ro=ro, i=i, pos=pos: e.matmul(
                        psZ[ab][:], lhsT=qT[ro:ro + 64, c, i * 128:(i + 1) * 128],
                        rhs=kTr[ro:ro + 64, c, pos * 128:pos * 128 + NKEY], start=True, stop=True),
                        reads=[B_qT, B_kTr], writes=[B_psZ[ab]])
                    sc.op("act", lambda e, ab=ab: e.activation(out=r_sb[ab][:], in_=psZ[ab][:], func=AF.Sigmoid, scale=-1.0),
                          reads=[B_psZ[ab]], writes=[B_r[ab]])
                    sc.op("dve", lambda e, ab=ab, msk=msk: e.tensor_tensor_scan(out=Pb[ab][:, 1:NKEY + 1], data0=r_sb[ab][:], data1=msk[:],
                                                                              initial=1.0, op0=ALU.mult, op1=ALU.max),
                          reads=[B_r[ab], B_w] + ([B_tmpf[1]] if msk is maskB else []), writes=[B_Pb[ab]])
                    sc.op("dve", lambda e, ab=ab: e.tensor_tensor(out=w_bf[ab][:], in0=Pb[ab][:, 0:NKEY], in1=Pb[ab][:, 1:NKEY + 1], op=ALU.subtract),
                          reads=[B_Pb[ab]], writes=[B_wbf[ab]])

                def att_B(n):
                    i, h = hts[n]
                    pos = GT - 1 - i
                    c = h // 2
                    ro = (h % 2) * 64
                    ab = n % 2
                    for j in range(1 + ATT_W):
                        sc.op("pe", lambda e, ab=ab, j=j: e.transpose(psW[ab][:, j * 128:(j + 1) * 128], w_bf[ab][:, j * 128:(j + 1) * 128], ident_b[:]),
                              reads=[B_wbf[ab], B_const], writes=[B_psW[ab]])
                    sc.op("act", lambda e, ab=ab: e.activation(out=wT[ab][:], in_=psW[ab][:], func=AF.Copy),
                          reads=[B_psW[ab]], writes=[B_wT[ab]])
                    for j in range(1 + ATT_W):
                        sc.op("pe", lambda e, ab=ab, j=j, c=c, ro=ro, pos=pos, h=h: e.matmul(
                            psY[ro:ro + 64, c * 128:(c + 1) * 128], lhsT=vr[:, pos + j, h * 64:(h + 1) * 64],
                            rhs=wT[ab][:, j * 128:(j + 1) * 128], start=(j == 0), stop=(j == ATT_W)),
                            reads=[B_vr, B_wT[ab]], writes=[B_psY])
                    if h == 7:
                        sc.op("act", lambda e, i=i: e.activation(out=yaT[:, :, i * 128:(i + 1) * 128], in_=psY[:].rearrange("p (a b) -> p a b", a=4), func=AF.Copy),
                              reads=[B_psY], writes=[B_yaT])

                att_A(0)
                for n in range(len(hts)):
                    if n + 1 < len(hts):
                        att_A(n + 1)
                    att_B(n)
                sc.op("act", lambda e: e.activation(out=kTr[:, :, GT * 128:(GT + ATT_W) * 128], in_=kTr[:, :, 0:ATT_W * 128], func=AF.Copy),
                      reads=[B_kTr], writes=[B_kTr])
                sc.op("dve", lambda e: e.tensor_copy(out=vr[:, GT:GT + ATT_W, :], in_=vr[:, 0:ATT_W, :]),
                      reads=[B_vr], writes=[B_vr])
                for m in range(4):
                    pt, bpt = next_psP()
                    for k in range(8):
                        sc.op("pe", lambda e, pt=pt, m=m, k=k: e.matmul(pt[:, 0:GW], lhsT=win_sb[:, k, C_U + m * 128:C_U + (m + 1) * 128], rhs=nT[:, k, :],
                                                                      start=(k == 0), stop=(k == 7)),
                              reads=[B_w, B_nT], writes=[bpt])
                    sc.op("act", lambda e, pt=pt, m=m: e.activation(out=uT[:, m, :], in_=pt[:, 0:GW], func=AF.Gelu_apprx_tanh),
                          reads=[bpt], writes=[B_uT])
                for i in range(GT):
                    pt, bpt = next_psP()
                    for g in range(8):
                        c = g // 2
                        ro = (g % 2) * 64
                        sc.op("pe", lambda e, pt=pt, g=g, c=c, ro=ro, i=i: e.matmul(pt[ro:ro + 64, c * 128:(c + 1) * 128], lhsT=vnb2[:, i, g * 64:(g + 1) * 64],
                                                                                  rhs=wct_sb[:, g, :], start=True, stop=True),
                              reads=[B_vnb, B_w], writes=[bpt])
                    sc.op("dve", lambda e, pt=pt: e.tensor_tensor(out=tmpf[0][:], in0=pt[:], in1=bsb[:].rearrange("p a b -> p (a b)"), op=ALU.add),
                          reads=[bpt, B_w], writes=[B_tmpf[0]])
                    sc.op("dve", lambda e, i=i: e.tensor_tensor(out=ybT[:, :, i * 128:(i + 1) * 128], in0=tmpf[0][:].rearrange("p (a b) -> p a b", a=4),
                                                              in1=uT[:, :, i * 128:(i + 1) * 128], op=ALU.mult),
                          reads=[B_tmpf[0], B_uT], writes=[B_ybT])

            def g_back1b(G):
                X, BX = grp_ctx(G)
                for m in range(8):
                    pAB, bA = next_psP()
                    pGG, bGa = next_psP()
                    bB = bA
                    bGb = bGa
                    pA, pB = pAB[:, 0:GW], pAB[:, GW:2 * GW]
                    pGa, pGb = pGG[:, 0:GW], pGG[:, GW:2 * GW]
                    for kc in range(4):
                        sc.op("pe", lambda e, pA=pA, kc=kc, m=m: e.matmul(pA[:], lhsT=wsba_sb[:, kc, m * 128:(m + 1) * 128], rhs=yaT[:, kc, :],
                                                                        start=(kc == 0), stop=(kc == 3)),
                              reads=[B_w, B_yaT], writes=[bA])
                    for kc in range(4):
                        sc.op("pe", lambda e, pB=pB, kc=kc, m=m: e.matmul(pB[:], lhsT=wsgu_sb[:, kc, m * 128:(m + 1) * 128], rhs=ybT[:, kc, :],
                                                                        start=(kc == 0), stop=(kc == 3)),
                              reads=[B_w, B_ybT], writes=[bB])
                    for k in range(8):
                        sc.op("pe", lambda e, pGa=pGa, k=k, m=m: e.matmul(pGa[:], lhsT=win_sb[:, k, C_GA + m * 128:C_GA + (m + 1) * 128], rhs=nT[:, k, :],
                                                                        start=(k == 0), stop=(k == 7)),
                              reads=[B_w, B_nT], writes=[bGa])
                    for k in range(8):
                        sc.op("pe", lambda e, pGb=pGb, k=k, m=m: e.matmul(pGb[:], lhsT=win_sb[:, k, C_GB + m * 128:C_GB + (m + 1) * 128], rhs=nT[:, k, :],
                                                                        start=(k == 0), stop=(k == 7)),
                              reads=[B_w, B_nT], writes=[bGb])
                    sc.op("act", lambda e, pGa=pGa: e.activation(out=tmpf[1][:, 0:GW], in_=pGa[:], func=AF.Sigmoid), reads=[bGa], writes=[B_tmpf[1]])
                    sc.op("act", lambda e, pGb=pGb: e.activation(out=tmpf[2][:, 0:GW], in_=pGb[:], func=AF.Sigmoid), reads=[bGb], writes=[B_tmpf[2]])
                    sc.op("dve", lambda e, pA=pA: e.tensor_tensor(out=tmpf[1][:, 0:GW], in0=tmpf[1][:, 0:GW], in1=pA[:], op=ALU.mult),
                          reads=[bA, B_tmpf[1]], writes=[B_tmpf[1]])
                    sc.op("dve", lambda e, pB=pB: e.tensor_tensor(out=tmpf[2][:, 0:GW], in0=tmpf[2][:, 0:GW], in1=pB[:], op=ALU.mult),
                          reads=[bB, B_tmpf[2]], writes=[B_tmpf[2]])
                    sc.op("dve", lambda e, m=m: e.tensor_tensor(out=mT[:, m, :], in0=tmpf[1][:, 0:GW], in1=tmpf[2][:, 0:GW], op=ALU.add),
                          reads=[B_tmpf[1], B_tmpf[2]], writes=[B_mT])

            def g_back2(G):
                X, BX = grp_ctx(G)
                sc.op("dve", lambda e: e.memset(stat[:, 20:24], 0.0), writes=[B_ss])
                def w1(i):
                    for half in range(2):
                        pt, bpt = next_psP()
                        for k in range(8):
                            sc.op("pe", lambda e, pt=pt, i=i, k=k, half=half: e.matmul(pt[:], lhsT=mT[:, k, i * 128:(i + 1) * 128],
                                                                                     rhs=wout_sb[:, k, half * 512:(half + 1) * 512],
                                                                                     start=(k == 0), stop=(k == 7)),
                                  reads=[B_w, B_mT], writes=[bpt])
                        sc.op("dve", lambda e, pt=pt, i=i, half=half: e.tensor_tensor(out=X[:, i, half * 512:(half + 1) * 512], in0=pt[:],
                                                                                    in1=X[:, i, half * 512:(half + 1) * 512], op=ALU.add),
                              reads=[bpt, BX], writes=[BX])
                    sc.op("act", lambda e, i=i: e.activation(out=vn[:].bitcast(BF16), in_=X[:, i, :], func=AF.Square, accum_out=stat[:, 20 + i:21 + i]),
                          reads=[BX, B_ss], writes=[B_vn, B_ss])


                def n2(i):
                    T = G * GT + i
                    sc.op("act", lambda e, i=i: e.activation(out=stat[:, 24 + i:25 + i], in_=stat[:, 20 + i:21 + i], func=AF.Sqrt, scale=1.0 / D, bias=EPS),
                          reads=[B_ss], writes=[B_ss])
                    sc.op("dve", lambda e, i=i: e.reciprocal(out=stat[:, 28 + i:29 + i], in_=stat[:, 24 + i:25 + i]), reads=[B_ss], writes=[B_rstd])
                    sc.op("dve", lambda e, i=i: e.scalar_tensor_tensor(out=n2f[:], in0=X[:, i, :], scalar=stat[:, 28 + i:29 + i], in1=af_row[:],
                                                                     op0=ALU.mult, op1=ALU.mult),
                          reads=[BX, B_rstd, B_w], writes=[B_n2f])
                    sc.op("dve", lambda e: e.tensor_tensor(out=n2f[:], in0=n2f[:], in1=shf_row[:], op=ALU.add),
                          reads=[B_n2f, B_w], writes=[B_n2f])
                    sc.op("act", lambda e: e.activation(out=n2b[:], in_=n2f[:], func=AF.Copy), reads=[B_n2f], writes=[B_n2b])
                    sc.dma("sp", lambda e, T=T: e.dma_start(out=n2_d.ap()[T * 128:(T + 1) * 128, :], in_=n2b[:]),
                           reads=[B_n2b], writes=[B_n2d], key="n2st")

                def router(i):
                    T = G * GT + i
                    sc.op("dve", lambda e: e.tensor_tensor(out=n2lo, in0=n2f[:], in1=n2b[:], op=ALU.subtract),
                          reads=[B_n2f, B_n2b], writes=[B_tmpf[0]])
                    for (srcT, dstT, bsrc) in ((n2b[:], hiT, B_n2b), (n2lo, loT, B_tmpf[0])):
                        for hlf in range(2):
                            for kk in range(4):
                                k = hlf * 4 + kk
                                sc.op("pe", lambda e, hlf=hlf, kk=kk, k=k, srcT=srcT: e.transpose(psTW[hlf][:, kk * 128:(kk + 1) * 128],
                                                                                                srcT[:, k * 128:(k + 1) * 128], ident_b[:]),
                                      reads=[bsrc, B_const], writes=[B_psT[hlf], B_psW[hlf]])
                        sc.op("act", lambda e, dstT=dstT: e.activation(out=dstT[:, 0:4, :], in_=psTW[0][:].rearrange("p (a b) -> p a b", a=4), func=AF.Copy),
                              reads=[B_psT[0], B_psW[0]], writes=[B_n2T])
                        sc.op("dve", lambda e, dstT=dstT: e.tensor_copy(out=dstT[:, 4:8, :], in_=psTW[1][:].rearrange("p (a b) -> p a b", a=4)),
                              reads=[B_psT[1], B_psW[1]], writes=[B_n2T])
                    pt, bpt = next_psP()
                    terms = [(hiT, wr_hi), (hiT, wr_lo), (loT, wr_hi)]
                    for ti, (aT, wv) in enumerate(terms):
                        for k in range(8):
                            sc.op("pe", lambda e, pt=pt, k=k, aT=aT, wv=wv, ti=ti: e.matmul(pt[:, 0:36], lhsT=aT[:, k, :], rhs=wv[:, k, :],
                                                                                         start=(ti == 0 and k == 0), stop=(ti == 2 and k == 7)),
                                  reads=[B_n2T, B_w], writes=[bpt])
                    sc.op("dve", lambda e, pt=pt, T=T: e.tensor_tensor(out=logits[:, T, :], in0=pt[:, 0:36], in1=br_row[:], op=ALU.add),
                          reads=[bpt, B_w], writes=[B_logits])


                assert GT == 2
                w1(0)
                n2(0)
                w1(1)
                sc.dma("sp", lambda e, G=G: e.dma_start(out=h1_d.ap()[G * GW:(G + 1) * GW, :].rearrange("(i p) d -> p i d", p=128), in_=X[:]),
                       reads=[BX], writes=[B_h1d], key="h1st")
                router(0)
                n2(1)
                router(1)

            g_load(0)
            g_front_a(0)
            g_front_b(0)
            for G in range(n_groups):
                if G + 1 < n_groups:
                    g_load(G + 1)
                g_back1a(G)
                if G + 1 < n_groups:
                    g_front_a(G + 1)
                g_back1b(G)
                if G + 1 < n_groups:
                    g_front_b(G + 1)
                g_back2(G)
            if debug:
                for nm, t_, bb, shp, dt2 in (("nT", nT, B_nT, [128, 8 * GW], BF16), ("qT", qT, B_qT, [128, 4 * GW], BF16),
                                             ("uT", uT, B_uT, [128, 4 * GW], F32), ("yaT", yaT, B_yaT, [128, 4 * GW], BF16),
                                             ("ybT", ybT, B_ybT, [128, 4 * GW], BF16), ("mT", mT, B_mT, [128, 8 * GW], BF16),
                                             ("kTr", kTr, B_kTr, [128, 4 * NSLOT * 128], BF16), ("vr", vr, B_vr, [128, NSLOT * 512], BF16),
                                             ("vs", vs, B_vs, [128, GT * 512], F32), ("amc", am_col, B_w, [128, 8], F32),
                                             ("woutb", wout_sb, B_w, [128, 8 * D], BF16),
                                             ("ktm", k_tm, B_ktm, [128, GT * 512], BF16), ("vtm", v_tm, B_vtm, [128, GT * 512], BF16),
                                             ("Jb", J_b, B_const, [128, 128], BF16)):
                    dd = nc.dram_tensor("dbg_" + nm, shp, dt2, kind="ExternalOutput")
                    src = t_[:].rearrange("p a b -> p (a b)") if len(t_.shape) == 3 else t_[:]
                    sc.dma("sp", lambda e, dd=dd, src=src: e.dma_start(out=dd.ap(), in_=src), reads=[bb], key="dbg")
            sc.barrier(exclude=("D_wconv_sw",))
            sc.emit(final=(stop == 'p1'))
            if stop == 'p1':
                return nc

        if debug:
            sc.dma("sp", lambda e: e.dma_start(out=dbg_logits_d.ap(), in_=logits[:].rearrange("p a b -> p (a b)")),
                   reads=[B_logits], key="dbg")

        if do_moe:
            _moe_phases(nc, sc, locals(), stop)
        else:
            _final_only(nc, sc, locals())

    return nc


def _final_only(nc, sc, L):
    raise NotImplementedError


def _moe_phases(nc, sc, L, stop=None):
    logits = L["logits"]; B_logits = L["B_logits"]; B_const = L["B_const"]; B_rows = L["B_rows"]
    ident_f = L["ident_f"]; ident_b = L["ident_b"]
    mod_d = L["mod_d"]; gfin_d = L["gfin_d"]
    h1_d = L["h1_d"]; n2_d = L["n2_d"]; yb_d = L["yb_d"]; tok_d = L["tok_d"]; out_d = L["out_d"]
    wegb_d = L["wegb_d"]; weub_d = L["weub_d"]; wedb_d = L["wedb_d"]
    B_wexp = L["B_wexp"]; B_h1d = L["B_h1d"]; B_n2d = L["B_n2d"]
    tris_c = L["tris_c"]; tokid_c = L["tokid_c"]; jrow_c = L["jrow_c"]; pidx_c = L["pidx_c"]
    debug = L["debug"]

    es2 = ExitStack()
    with es2:
        P = es2.enter_context
        dest_i = P(nc.sbuf_tensor("dest_i", [128, NT, 2], I32))
        tw = P(nc.sbuf_tensor("tw", [128, NT, 2], F32))
        tokidx = P(nc.sbuf_tensor("tokidx", [128, NBLK], I32))
        blk_i = P(nc.sbuf_tensor("blk_i", [1, NBLK], I32))
        widx = P(nc.sbuf_tensor("widx", [128, NBLK], I32))
        B_dest = Buf("dest_i"); B_tw = Buf("tw"); B_tokidx = Buf("tokidx"); B_blk = Buf("blk_i")
        B_tokd = Buf("tok_d")

        p2 = ExitStack()
        with p2:
            Q = p2.enter_context

            def sb2(name, shape, dtype):
                return Q(nc.sbuf_tensor(name, shape, dtype))
            gmax = sb2("gmax", [128, NT], F32)
            goh = sb2("goh", [128, NT, 4], F32)
            gsh = sb2("gsh", [128, NT, 4], F32)
            gsum = sb2("gsum", [128, NT], F32)
            gw = sb2("gw", [128, NT], F32)
            el = sb2("el", [128, NT, 8], F32)
            el2 = sb2("el2", [128, NT, 8], F32)
            elt = sb2("elt", [128, NT, 8], F32)
            m1 = sb2("m1", [128, NT], F32)
            m2 = sb2("m2", [128, NT], F32)
            oh1 = sb2("oh1", [128, NT, 8], F32)
            oh2 = sb2("oh2", [128, NT, 8], F32)
            dm = sb2("dm", [128, NT], F32)
            w1 = sb2("w1", [128, NT], F32)
            A1 = sb2("A1", [128, NT, 32], F32)
            A2 = sb2("A2", [128, NT, 32], F32)
            A1T = sb2("A1T", [32, S], F32)
            A2T = sb2("A2T", [32, S], F32)
            AT = sb2("AT", [32, S], F32)
            CT = sb2("CT", [32, S], F32)
            sm = sb2("sm", [32, 16], F32)
            tris = sb2("tris", [32, 32], F32)
            ones32 = sb2("ones32", [32, 1], F32)
            jrow = sb2("jrow", [32, NBLK], F32)
            cmpb = sb2("cmpb", [32, NBLK], F32)
            blk_f = sb2("blk_f", [1, NBLK], F32)
            dest_f = sb2("dest_f", [128, NT, 2], F32)
            dmod = sb2("dmod", [128, NT, 2], F32)
            off_f = sb2("off_f", [128, NT, 2], F32)
            off_i = sb2("off_i", [128, NT, 2], I32)
            dmod_i = sb2("dmod_i", [128, NT, 2], I32)
            djv_i = sb2("djv_i", [128, NT, 2], I32)
            smi = sb2("smi", [32, 4], I32)
            tokid = sb2("tokid", [128, NT], I32)
            padidx = sb2("padidx", [128, NBLK], I32)
            ones1 = sb2("ones1", [1, 128], F32)
            eq_f = sb2("eq_f", [1, NBLK], F32)
            widx_f = sb2("widx_f", [128, NBLK], F32)
            pidx_f = sb2("pidx_f", [128, NBLK], F32)
            pidx_i = sb2("pidx_i", [128, NBLK], I32)
            psA_ = [Q(nc.psum_tensor(f"psA2_{i}", [128, 512], F32)) for i in range(2)]
            psA2_ = [Q(nc.psum_tensor(f"psB2_{i}", [128, 512], F32)) for i in range(2)]
            psS = Q(nc.psum_tensor("psS", [128, 512], F32))
            B = {n: Buf(n) for n in ("g", "el", "oh", "A", "AT", "sm", "c2", "dest", "ps0", "ps1", "pb0", "pb1", "psS", "off")}

            def bc3(t, inner, n_inner):
                a = t
                return bass.AP(tensor=a.tensor, offset=a.offset, ap=[list(a.ap[0]), list(a.ap[1]), [0, n_inner]])

            sc.dma("sp", lambda e: e.dma_start(out=tris[:], in_=tris_c.ap()), writes=[B["c2"]], key="p2c_11")
            sc.dma("sp", lambda e: e.dma_start(out=jrow[:], in_=jrow_c.ap()), writes=[B["c2"]], key="p2c_12")
            sc.dma("sp", lambda e: e.dma_start(out=tokid[:], in_=tokid_c.ap()), writes=[B["c2"]], key="p2c_13")
            sc.dma("sp", lambda e: e.dma_start(out=pidx_i[:], in_=pidx_c.ap()), writes=[B["c2"]], key="p2c_14")
            sc.op("dve", lambda e: e.tensor_copy(out=pidx_f[:], in_=pidx_i[:]), reads=[B["c2"]], writes=[B["c2"]])
            sc.op("dve", lambda e: e.memset(ones32[:], 1.0), writes=[B["c2"]])
            sc.op("dve", lambda e: e.memset(padidx[:], S), writes=[B["c2"]])
            sc.dma("sp", lambda e: e.dma_start(out=tok_d.ap().rearrange("(p j) o -> p (j o)", p=128), in_=padidx[:]),
                   reads=[B["c2"]], writes=[B_tokd], key="p2c_15")

            gl = logits[:, :, 0:4]
            sc.op("dve", lambda e: e.tensor_reduce(out=gmax[:], in_=gl, axis=AX.X, op=ALU.max), reads=[B_logits], writes=[B["g"]])
            sc.op("dve", lambda e: e.tensor_tensor(out=goh[:], in0=gl, in1=bc3(gmax[:], 0, 4), op=ALU.is_ge), reads=[B_logits, B["g"]], writes=[B["g"]])
            sc.op("dve", lambda e: e.tensor_tensor(out=gsh[:], in0=gl, in1=bc3(gmax[:], 0, 4), op=ALU.subtract), reads=[B_logits, B["g"]], writes=[B["g"]])
            sc.op("act", lambda e: e.activation(out=gsh[:], in_=gsh[:], func=AF.Exp), reads=[B["g"]], writes=[B["g"]])
            sc.op("dve", lambda e: e.tensor_reduce(out=gsum[:], in_=gsh[:], axis=AX.X, op=ALU.add), reads=[B["g"]], writes=[B["g"]])
            sc.op("dve", lambda e: e.reciprocal(out=gw[:], in_=gsum[:]), reads=[B["g"]], writes=[B["g"]])
            for g in range(4):
                src = logits[:, :, 4 + 8 * g:12 + 8 * g]
                gsel = goh[:, :, g]
                if g == 0:
                    sc.op("dve", lambda e, src=src, gsel=gsel: e.tensor_tensor(out=el[:], in0=src, in1=bc3(gsel, 0, 8), op=ALU.mult),
                          reads=[B_logits, B["g"]], writes=[B["el"]])
                else:
                    sc.op("dve", lambda e, src=src, gsel=gsel: e.tensor_tensor(out=elt[:], in0=src, in1=bc3(gsel, 0, 8), op=ALU.mult),
                          reads=[B_logits, B["g"]], writes=[B["el"]])
                    sc.op("dve", lambda e: e.tensor_tensor(out=el[:], in0=el[:], in1=elt[:], op=ALU.add), reads=[B["el"]], writes=[B["el"]])
            sc.op("dve", lambda e: e.tensor_reduce(out=m1[:], in_=el[:], axis=AX.X, op=ALU.max), reads=[B["el"]], writes=[B["oh"]])
            sc.op("dve", lambda e: e.tensor_tensor(out=oh1[:], in0=el[:], in1=bc3(m1[:], 0, 8), op=ALU.is_ge), reads=[B["el"], B["oh"]], writes=[B["oh"]])
            sc.op("dve", lambda e: e.scalar_tensor_tensor(out=el2[:], in0=oh1[:], scalar=-1e30, in1=el[:], op0=ALU.mult, op1=ALU.add),
                  reads=[B["el"], B["oh"]], writes=[B["oh"]])
            sc.op("dve", lambda e: e.tensor_reduce(out=m2[:], in_=el2[:], axis=AX.X, op=ALU.max), reads=[B["oh"]], writes=[B["oh"]])
            sc.op("dve", lambda e: e.tensor_tensor(out=oh2[:], in0=el2[:], in1=bc3(m2[:], 0, 8), op=ALU.is_ge), reads=[B["oh"]], writes=[B["oh"]])
            sc.op("dve", lambda e: e.tensor_tensor(out=dm[:], in0=m1[:], in1=m2[:], op=ALU.subtract), reads=[B["oh"]], writes=[B["oh"]])
            sc.op("act", lambda e: e.activation(out=w1[:], in_=dm[:], func=AF.Sigmoid), reads=[B["oh"]], writes=[B["oh"]])
            sc.op("dve", lambda e: e.tensor_tensor(out=tw[:, :, 0], in0=w1[:], in1=gw[:], op=ALU.mult), reads=[B["oh"], B["g"]], writes=[B_tw])
            sc.op("dve", lambda e: e.tensor_tensor(out=tw[:, :, 1], in0=gw[:], in1=tw[:, :, 0], op=ALU.subtract), reads=[B["g"], B_tw], writes=[B_tw])
            for g in range(4):
                gsel = goh[:, :, g]
                sc.op("dve", lambda e, g=g, gsel=gsel: e.tensor_tensor(out=A1[:, :, g * 8:(g + 1) * 8], in0=oh1[:], in1=bc3(gsel, 0, 8), op=ALU.mult),
                      reads=[B["oh"], B["g"]], writes=[B["A"]])
                sc.op("dve", lambda e, g=g, gsel=gsel: e.tensor_tensor(out=A2[:, :, g * 8:(g + 1) * 8], in0=oh2[:], in1=bc3(gsel, 0, 8), op=ALU.mult),
                      reads=[B["oh"], B["g"]], writes=[B["A"]])
            for r in range(NT // 4):
                for (Asrc, Adst, pss, bn) in ((A1, A1T, psA_, "ps"), (A2, A2T, psA2_, "pb")):
                    pb_ = r % 2
                    pt = pss[pb_]
                    bpt = B[f"{bn}{pb_}"]
                    for q in range(4):
                        T = r * 4 + q
                        sc.op("pe", lambda e, pt=pt, q=q, T=T, Asrc=Asrc: e.transpose(pt[0:32, q * 128:(q + 1) * 128], Asrc[:, T, :], ident_f[:]),
                              reads=[B["A"], B_const], writes=[bpt])
                    sc.op("act", lambda e, pt=pt, r=r, Adst=Adst: e.activation(out=Adst[:, r * 512:(r + 1) * 512], in_=pt[0:32, :], func=AF.Copy),
                          reads=[bpt], writes=[B["AT"]])
            sc.op("dve", lambda e: e.tensor_tensor(out=AT[:], in0=A1T[:], in1=A2T[:], op=ALU.add), reads=[B["AT"]], writes=[B["AT"]])
            sc.op("dve", lambda e: e.tensor_tensor_scan(out=CT[:], data0=AT[:], data1=AT[:], initial=0.0, op0=ALU.add, op1=ALU.max),
                  reads=[B["AT"]], writes=[B["AT"]])
            sc.op("dve", lambda e: e.tensor_copy(out=sm[:, 0:1], in_=CT[:, S - 1:S]), reads=[B["AT"]], writes=[B["sm"]])
            sc.op("dve", lambda e: e.tensor_scalar(out=sm[:, 1:2], in0=sm[:, 0:1], scalar1=float(EPAD - 1), scalar2=None, op0=ALU.add), reads=[B["sm"]], writes=[B["sm"]])
            sc.op("dve", lambda e: e.tensor_copy(out=smi[:, 0:1], in_=sm[:, 1:2]), reads=[B["sm"]], writes=[B["sm"]])
            sc.op("dve", lambda e: e.tensor_single_scalar(out=smi[:, 1:2], in_=smi[:, 0:1], scalar=8, op=ALU.arith_shift_right), reads=[B["sm"]], writes=[B["sm"]])
            sc.op("dve", lambda e: e.tensor_copy(out=sm[:, 2:3], in_=smi[:, 1:2]), reads=[B["sm"]], writes=[B["sm"]])
            sc.op("dve", lambda e: e.tensor_scalar(out=sm[:, 3:4], in0=sm[:, 2:3], scalar1=float(EPAD), scalar2=None, op0=ALU.mult), reads=[B["sm"]], writes=[B["sm"]])
            sc.op("pe", lambda e: e.matmul(psS[0:32, 0:1], lhsT=tris[:], rhs=sm[:, 3:4], start=True, stop=True), reads=[B["sm"], B["c2"]], writes=[B["psS"]])
            sc.op("dve", lambda e: e.tensor_copy(out=sm[:, 4:5], in_=psS[0:32, 0:1]), reads=[B["psS"]], writes=[B["sm"]])
            sc.op("dve", lambda e: e.tensor_tensor(out=sm[:, 5:6], in0=sm[:, 4:5], in1=sm[:, 3:4], op=ALU.add), reads=[B["sm"]], writes=[B["sm"]])
            sc.op("dve", lambda e: e.tensor_scalar(out=cmpb[:], in0=jrow[:], scalar1=sm[:, 5:6], scalar2=None, op0=ALU.is_ge),
                  reads=[B["sm"], B["c2"]], writes=[B["sm"]])
            sc.op("pe", lambda e: e.matmul(psS[0:1, 128:128 + NBLK], lhsT=ones32[:], rhs=cmpb[:], start=True, stop=True),
                  reads=[B["sm"], B["c2"]], writes=[B["psS"]])
            sc.op("dve", lambda e: e.tensor_scalar(out=blk_f[:], in0=psS[0:1, 128:128 + NBLK], scalar1=31.0, scalar2=None, op0=ALU.min),
                  reads=[B["psS"]], writes=[B["sm"]])
            sc.op("dve", lambda e: e.tensor_copy(out=blk_i[:], in_=blk_f[:]), reads=[B["sm"]], writes=[B_blk])
            sc.op("dve", lambda e: e.memset(ones1[:], 1.0), writes=[B["c2"]])
            sc.op("dve", lambda e: e.memset(eq_f[:], 0.0), writes=[B["sm"]])
            sc.op("dve", lambda e: e.tensor_tensor(out=eq_f[0:1, 2:NBLK], in0=blk_f[0:1, 2:NBLK], in1=blk_f[0:1, 0:NBLK - 2], op=ALU.is_equal),
                  reads=[B["sm"]], writes=[B["sm"]])
            sc.op("pe", lambda e: e.matmul(psA_[0][:, 0:NBLK], lhsT=ones1[:], rhs=blk_f[:], start=True, stop=True),
                  reads=[B["sm"], B["c2"]], writes=[B["ps0"]])
            sc.op("pe", lambda e: e.matmul(psA_[0][:, 256:256 + NBLK], lhsT=ones1[:], rhs=eq_f[:], start=True, stop=True),
                  reads=[B["sm"], B["c2"]], writes=[B["ps0"]])
            sc.op("dve", lambda e: e.scalar_tensor_tensor(out=widx_f[:], in0=psA_[0][:, 0:NBLK], scalar=128.0, in1=pidx_f[:],
                                                          op0=ALU.mult, op1=ALU.add),
                  reads=[B["ps0"], B["c2"]], writes=[B["sm"]])
            sc.op("dve", lambda e: e.scalar_tensor_tensor(out=widx_f[:], in0=psA_[0][:, 256:256 + NBLK], scalar=0.0, in1=widx_f[:],
                                                          op0=ALU.mult, op1=ALU.add),
                  reads=[B["ps0"], B["sm"]], writes=[B["sm"]])
            sc.op("dve", lambda e: e.tensor_copy(out=widx[:], in_=widx_f[:]), reads=[B["sm"]], writes=[B_blk])
            sc.op("dve", lambda e: e.tensor_tensor(out=CT[:], in0=CT[:], in1=AT[:], op=ALU.subtract), reads=[B["AT"]], writes=[B["AT"]])
            sc.op("dve", lambda e: e.tensor_scalar(out=CT[:], in0=CT[:], scalar1=sm[:, 4:5], scalar2=None, op0=ALU.add),
                  reads=[B["AT"], B["sm"]], writes=[B["AT"]])
            sc.op("dve", lambda e: e.tensor_tensor(out=A1T[:], in0=A1T[:], in1=CT[:], op=ALU.mult), reads=[B["AT"]], writes=[B["AT"]])
            sc.op("dve", lambda e: e.tensor_tensor(out=A2T[:], in0=A2T[:], in1=CT[:], op=ALU.mult), reads=[B["AT"]], writes=[B["AT"]])
            for T in range(NT):
                for k_, Dk in ((0, A1T), (1, A2T)):
                    col = 384 + T * 2 + k_
                    sc.op("pe", lambda e, T=T, Dk=Dk, col=col: e.matmul(psS[:, col:col + 1], lhsT=Dk[:, T * 128:(T + 1) * 128], rhs=ones32[:],
                                                                       start=True, stop=True),
                          reads=[B["AT"], B["c2"]], writes=[B["psS"]])
            sc.op("dve", lambda e: e.tensor_copy(out=dest_f[:].rearrange("p a b -> p (a b)"), in_=psS[:, 384:384 + 2 * NT]),
                  reads=[B["psS"]], writes=[B["dest"]])
            sc.op("dve", lambda e: e.tensor_copy(out=dest_i[:], in_=dest_f[:]), reads=[B["dest"]], writes=[B_dest])
            sc.op("dve", lambda e: e.tensor_single_scalar(out=dmod_i[:], in_=dest_i[:], scalar=127, op=ALU.bitwise_and), reads=[B_dest], writes=[B["off"]])
            sc.op("dve", lambda e: e.tensor_single_scalar(out=djv_i[:], in_=dest_i[:], scalar=7, op=ALU.arith_shift_right), reads=[B_dest], writes=[B["off"]])
            sc.op("dve", lambda e: e.tensor_copy(out=dmod[:], in_=dmod_i[:]), reads=[B["off"]], writes=[B["off"]])
            sc.op("dve", lambda e: e.tensor_copy(out=off_f[:], in_=djv_i[:]), reads=[B["off"]], writes=[B["off"]])
            sc.op("dve", lambda e: e.scalar_tensor_tensor(out=off_f[:], in0=dmod[:], scalar=float(NBLK), in1=off_f[:], op0=ALU.mult, op1=ALU.add),
                  reads=[B["off"]], writes=[B["off"]])
            sc.op("dve", lambda e: e.tensor_copy(out=off_i[:], in_=off_f[:]), reads=[B["off"]], writes=[B["off"]])
            for T in range(NT):
                for k_ in range(2):
                    sc.dma("pool", lambda e, T=T, k_=k_: e.indirect_dma_start(
                        out=tok_d.ap(), out_offset=bass.IndirectOffsetOnAxis(ap=off_i[:, T, k_:k_ + 1], axis=0),
                        in_=tokid[:, T:T + 1], in_offset=None),
                        reads=[B["off"], B["c2"]], writes=[B_tokd], key="scat")
            sc.dma("sp", lambda e: e.dma_start(out=tokidx[:], in_=tok_d.ap().rearrange("(p j) o -> p (j o)", p=128)),
                   reads=[B_tokd], writes=[B_tokidx], key="tokidx")
            if debug:
                sc.dma("sp", lambda e: e.dma_start(out=L["dbg_dest_d"].ap(), in_=dest_f[:].rearrange("p a b -> p (a b)")), reads=[B["dest"]], key="dbg")
                sc.dma("sp", lambda e: e.dma_start(out=L["dbg_tw_d"].ap(), in_=tw[:].rearrange("p a b -> p (a b)")), reads=[B_tw], key="dbg")
                sc.dma("sp", lambda e: e.dma_start(out=L["dbg_blk_d"].ap(), in_=blk_i[:]), reads=[B_blk], key="dbg")
            sc.barrier()
            sc.emit(final=(stop == 'p2'))
            if stop == 'p2':
                return

        p3 = ExitStack()
        with p3:
            Q = p3.enter_context
            wg = [Q(nc.sbuf_tensor(f"wg{i}", [128, 8, 512], BF16)) for i in range(3)]
            wu = [Q(nc.sbuf_tensor(f"wu{i}", [128, 8, 512], BF16)) for i in range(3)]
            wd = [Q(nc.sbuf_tensor(f"wd{i}", [128, 4, D], BF16)) for i in range(3)]
            xgat = [Q(nc.sbuf_tensor(f"xgat{i}", [128, D], BF16)) for i in range(2)]
            nTb = [Q(nc.sbuf_tensor(f"nTb{i}", [128, 8, 128], BF16)) for i in range(2)]
            slu = Q(nc.sbuf_tensor("slu", [128, 512], F32))
            hidT = [Q(nc.sbuf_tensor(f"hidT{i}", [128, 4, 128], BF16)) for i in range(2)]
            yo = [Q(nc.sbuf_tensor(f"yo{i}", [128, D], F32)) for i in range(2)]
            psXa = Q(nc.psum_tensor("psXa", [128, 512], BF16))
            psXb = Q(nc.psum_tensor("psXb", [128, 512], BF16))
            psG = [Q(nc.psum_tensor(f"psG{i}", [128, 512], F32)) for i in range(2)]
            psU = [Q(nc.psum_tensor(f"psU{i}", [128, 512], F32)) for i in range(2)]
            psO = [Q(nc.psum_tensor(f"psO{i}", [128, 512], F32)) for i in range(2)]
            B_wg = [Buf("wg0"), Buf("wg1"), Buf("wg2")]; B_xgat = [Buf("xgat0"), Buf("xgat1")]; B_nTb = [Buf("nTb0"), Buf("nTb1")]
            B_slu = Buf("slu"); B_hidT = [Buf("hidT0"), Buf("hidT1")]; B_yo = [Buf("yo0"), Buf("yo1")]
            B_psX = Buf("psX"); B_psG = [Buf("psG0"), Buf("psG1")]; B_psU = [Buf("psU0"), Buf("psU1")]; B_psO = [Buf(f"psO{i}") for i in range(2)]
            B_ybd = Buf("yb_d")

            def p3_A(j):
                sl = j % 2
                w3 = (j // 2) % 3
                for (wt, wsrc, nm) in (((wg, wegb_d, "wg"), (wu, weub_d, "wu"), (wd, wedb_d, "wd")) if j % 2 == 0 else ()):
                    sc.dma("pool", lambda e, j=j, w3=w3, wt=wt, wsrc=wsrc: e.indirect_dma_start(
                        out=wt[w3][:].rearrange("p a b -> p (a b)"), out_offset=None, in_=wsrc.ap(),
                        in_offset=bass.IndirectOffsetOnAxis(ap=widx[:, j:j + 1], axis=0)),
                        reads=[B_blk, B_wexp], writes=[B_wg[w3]], key=f"{nm}{w3}")
                sc.dma("pool", lambda e, j=j, sl=sl: e.indirect_dma_start(
                    out=xgat[sl][:], out_offset=None, in_=n2_d.ap(),
                    in_offset=bass.IndirectOffsetOnAxis(ap=tokidx[:, j:j + 1], axis=0)),
                    reads=[B_tokidx, B_n2d], writes=[B_xgat[sl]], key=f"xgat{sl}")
                for k in range(8):
                    pxt = psXa if k < 4 else psXb
                    sc.op("pe", lambda e, k=k, sl=sl, pxt=pxt: e.transpose(pxt[:, (k % 4) * 128:(k % 4 + 1) * 128], xgat[sl][:, k::8], ident_b[:]),
                          reads=[B_xgat[sl], B_const], writes=[B_psX])
                sc.op("act", lambda e, sl=sl: e.activation(out=nTb[sl][:, 0:4, :], in_=psXa[:].rearrange("p (a b) -> p a b", a=4), func=AF.Copy),
                      reads=[B_psX], writes=[B_nTb[sl]])
                sc.op("dve", lambda e, sl=sl: e.tensor_copy(out=nTb[sl][:, 4:8, :], in_=psXb[:].rearrange("p (a b) -> p a b", a=4)),
                      reads=[B_psX], writes=[B_nTb[sl]])
                for hc in range(4):
                    for k in range(8):
                        sc.op("pe", lambda e, hc=hc, k=k, sl=sl, w3=w3: e.matmul(psG[sl][:, hc * 128:(hc + 1) * 128], lhsT=wg[w3][:, k, hc::4],
                                                                        rhs=nTb[sl][:, k, :], start=(k == 0), stop=(k == 7)),
                              reads=[B_wg[w3], B_nTb[sl]], writes=[B_psG[sl]])
                for hc in range(4):
                    for k in range(8):
                        sc.op("pe", lambda e, hc=hc, k=k, sl=sl, w3=w3: e.matmul(psU[sl][:, hc * 128:(hc + 1) * 128], lhsT=wu[w3][:, k, hc::4],
                                                                        rhs=nTb[sl][:, k, :], start=(k == 0), stop=(k == 7)),
                              reads=[B_wg[w3], B_nTb[sl]], writes=[B_psU[sl]])
                sc.op("act", lambda e, sl=sl: e.activation(out=slu[:], in_=psG[sl][:], func=AF.Silu), reads=[B_psG[sl]], writes=[B_slu])
                sc.op("dve", lambda e, sl=sl: e.tensor_tensor(out=hidT[sl][:].rearrange("p a b -> p (a b)"), in0=slu[:], in1=psU[sl][:], op=ALU.mult),
                      reads=[B_slu, B_psU[sl]], writes=[B_hidT[sl]])

            def p3_B(j):
                sl = j % 2
                w3 = (j // 2) % 3
                for half in range(2):
                    po = psO[half]
                    bpo = B_psO[half]
                    for hc in range(4):
                        sc.op("pe", lambda e, po=po, hc=hc, half=half, sl=sl, w3=w3: e.matmul(po[:], lhsT=hidT[sl][:, hc, :],
                                                                                     rhs=wd[w3][:, hc, half * 512:(half + 1) * 512],
                                                                                     start=(hc == 0), stop=(hc == 3)),
                              reads=[B_hidT[sl], B_wg[w3]], writes=[bpo])
                sc.op("act", lambda e, sl=sl: e.activation(out=yo[sl][:, 0:512], in_=psO[0][:], func=AF.Copy), reads=[B_psO[0]], writes=[B_yo[sl]])
                sc.op("dve", lambda e, sl=sl: e.tensor_copy(out=yo[sl][:, 512:1024], in_=psO[1][:]), reads=[B_psO[1]], writes=[B_yo[sl]])
                sc.dma("sp", lambda e, j=j, sl=sl: e.dma_start(out=yb_d.ap()[j * 128:(j + 1) * 128, :], in_=yo[sl][:]),
                       reads=[B_yo[sl]], writes=[B_ybd], key=f"ybst{sl}")

            p3_A(0)
            for j in range(NBLK):
                if j + 1 < NBLK:
                    p3_A(j + 1)
                p3_B(j)
            sc.barrier()
            sc.emit(final=(stop == 'p3'))
            if stop == 'p3':
                return

        p4 = ExitStack()
        with p4:
            Q = p4.enter_context
            r1 = [Q(nc.sbuf_tensor(f"r1_{i}", [128, D], F32)) for i in range(2)]
            r2 = [Q(nc.sbuf_tensor(f"r2_{i}", [128, D], F32)) for i in range(2)]
            h1t = [Q(nc.sbuf_tensor(f"h1t{i}", [128, D], F32)) for i in range(2)]
            ot = [Q(nc.sbuf_tensor(f"ot{i}", [128, D], F32)) for i in range(2)]
            junk4 = Q(nc.sbuf_tensor("junk4", [128, D], F32))
            gtf_row = Q(nc.sbuf_tensor("gtf_row", [128, D], F32))
            gfin_row = Q(nc.sbuf_tensor("gfin_row", [128, D], F32))
            sc.dma("sp", lambda e: e.dma_start(out=gfin_row[:], in_=bass.AP(tensor=gfin_d.ap().tensor, offset=0, ap=[[0, 128], [1, D]])),
                   writes=[B_rows], key="c_gfin")
            sc.dma("sp", lambda e: e.dma_start(out=gtf_row[:], in_=bass.AP(tensor=mod_d.ap().tensor, offset=5 * D, ap=[[0, 128], [1, D]])),
                   writes=[B_rows], key="c_gtf")
            st4 = Q(nc.sbuf_tensor("st4", [128, 2 * NT + 2], F32))
            B_r1 = [Buf("r1_0"), Buf("r1_1")]; B_r2 = [Buf("r2_0"), Buf("r2_1")]; B_h1t = [Buf("h1t0"), Buf("h1t1")]
            B_ot = [Buf("ot0"), Buf("ot1")]; B_j4 = Buf("junk4"); B_st4 = Buf("st4")
            B_out = Buf("out")
            sc.op("dve", lambda e: e.memset(st4[:], 0.0), writes=[B_st4])
            def p4_A(T):
                sl = T % 2
                sc.dma("pool", lambda e, T=T, sl=sl: e.indirect_dma_start(
                    out=r1[sl][:], out_offset=None, in_=yb_d.ap(),
                    in_offset=bass.IndirectOffsetOnAxis(ap=dest_i[:, T, 0:1], axis=0)),
                    reads=[B_dest, B_ybd], writes=[B_r1[sl]], key=f"r1_{sl}")
                sc.dma("pool", lambda e, T=T, sl=sl: e.indirect_dma_start(
                    out=r2[sl][:], out_offset=None, in_=yb_d.ap(),
                    in_offset=bass.IndirectOffsetOnAxis(ap=dest_i[:, T, 1:2], axis=0)),
                    reads=[B_dest, B_ybd], writes=[B_r2[sl]], key=f"r2_{sl}")
                sc.dma("sp", lambda e, T=T, sl=sl: e.dma_start(out=h1t[sl][:], in_=h1_d.ap()[T * 128:(T + 1) * 128, :]),
                       reads=[B_h1d], writes=[B_h1t[sl]], key=f"h1ld{sl}")
                sc.op("act", lambda e, T=T, sl=sl: e.activation(out=r1[sl][:], in_=r1[sl][:], func=AF.Copy, scale=tw[:, T, 0:1]),
                      reads=[B_r1[sl], B_tw], writes=[B_r1[sl]])
                sc.op("dve", lambda e, T=T, sl=sl: e.scalar_tensor_tensor(out=r2[sl][:], in0=r2[sl][:], scalar=tw[:, T, 1:2], in1=r1[sl][:],
                                                                        op0=ALU.mult, op1=ALU.add),
                      reads=[B_r2[sl], B_r1[sl], B_tw], writes=[B_r2[sl]])
                sc.op("dve", lambda e, sl=sl: e.tensor_tensor(out=r2[sl][:], in0=r2[sl][:], in1=gtf_row[:], op=ALU.mult),
                      reads=[B_r2[sl], B_rows], writes=[B_r2[sl]])
                sc.op("dve", lambda e, sl=sl: e.tensor_tensor(out=h1t[sl][:], in0=h1t[sl][:], in1=r2[sl][:], op=ALU.add),
                      reads=[B_r2[sl], B_h1t[sl]], writes=[B_h1t[sl]])
                sc.op("act", lambda e, T=T, sl=sl: e.activation(out=junk4[:], in_=h1t[sl][:], func=AF.Square, accum_out=st4[:, 2 * T:2 * T + 1]),
                      reads=[B_h1t[sl], B_st4], writes=[B_j4, B_st4])

            def p4_B(T):
                sl = T % 2
                sc.op("act", lambda e, T=T: e.activation(out=st4[:, 2 * T + 1:2 * T + 2], in_=st4[:, 2 * T:2 * T + 1], func=AF.Sqrt, scale=1.0 / D, bias=EPS),
                      reads=[B_st4], writes=[B_st4])
                sc.op("dve", lambda e, T=T: e.reciprocal(out=st4[:, 2 * T:2 * T + 1], in_=st4[:, 2 * T + 1:2 * T + 2]), reads=[B_st4], writes=[B_st4])
                sc.op("act", lambda e, T=T, sl=sl: e.activation(out=ot[sl][:], in_=h1t[sl][:], func=AF.Copy, scale=st4[:, 2 * T:2 * T + 1]),
                      reads=[B_h1t[sl], B_st4], writes=[B_ot[sl]])
                sc.op("dve", lambda e, sl=sl: e.tensor_tensor(out=ot[sl][:], in0=ot[sl][:], in1=gfin_row[:], op=ALU.mult),
                      reads=[B_ot[sl], B_rows], writes=[B_ot[sl]])
                sc.dma("sp", lambda e, T=T, sl=sl: e.dma_start(out=out_d.ap()[T * 128:(T + 1) * 128, :], in_=ot[sl][:]),
                       reads=[B_ot[sl]], writes=[B_out], key=f"ost{sl}")

            p4_A(0)
            for T in range(NT):
                if T + 1 < NT:
                    p4_A(T + 1)
                p4_B(T)
            sc.emit(final=True)


_NC_CACHE = {}


def _get_nc(debug=False):
    if debug not in _NC_CACHE:
        _NC_CACHE[debug] = build_program(debug=debug)
    return _NC_CACHE[debug]


def make_in_maps(inputs, cores):
    f = lambda a: np.ascontiguousarray(np.asarray(a, dtype=np.float32))
    x = f(inputs["x"])
    c = f(inputs["c"])
    shared = {
        "gmix2": np.ascontiguousarray(f(inputs["g_mix"]).reshape(8, 128).T),
        "g_ffn": f(inputs["g_ffn"]).reshape(1, D),
        "g_final": f(inputs["g_final"]).reshape(1, D),
        "w_ada": f(inputs["w_ada"]).reshape(D, 6 * D),
        "b_ada": f(inputs["b_ada"]).reshape(1, 6 * D),
        "w_in": f(inputs["w_in"]).reshape(D, 4608),
        "w_sba_out": f(inputs["w_sba_out"]).reshape(512, D),
        "g_sgu": f(inputs["g_sgu"]).reshape(1, 512),
        "w_spatial": f(inputs["w_spatial"]).reshape(8, 128, 128),
        "b_spatial": f(inputs["b_spatial"]).reshape(8, 128),
        "w_sgu_out": f(inputs["w_sgu_out"]).reshape(512, D),
        "w_out": f(inputs["w_out"]).reshape(D, D),
        "w_r": np.ascontiguousarray(np.concatenate([f(inputs["w_router_group"]).reshape(D, 4),
                                                    f(inputs["w_router_expert"]).reshape(D, 32)], axis=1)),
        "b_r": np.ascontiguousarray(np.concatenate([f(inputs["b_router_group"]).reshape(1, 4),
                                                    f(inputs["b_router_expert"]).reshape(1, 32)], axis=1)),
        "w_eg": f(inputs["w_expert_gate"]).reshape(32, D, 512),
        "w_eu": f(inputs["w_expert_up"]).reshape(32, D, 512),
        "w_ed": f(inputs["w_expert_down"]).reshape(32, 512, D),
    }
    maps = []
    for b in cores:
        m = dict(shared)
        m["x"] = np.ascontiguousarray(x[b])
        m["c2"] = np.ascontiguousarray(c[b].reshape(8, 128).T)
        maps.append(m)
    return maps


def kernel(**inputs):
    nc = _get_nc(False)
    in_maps = make_in_maps(inputs, list(range(8)))
    res = run_bass_kernel_spmd(nc, in_maps, core_ids=list(range(8)))
    return np.stack([np.asarray(r["out"], dtype=np.float32) for r in res.results], axis=0)
```

```python
import numpy as np
from contextlib import ExitStack
import concourse.bass as bass
import concourse.mybir as mybir
from concourse.bass_utils import run_bass_kernel_spmd

F32 = mybir.dt.float32
BF16 = mybir.dt.bfloat16
I32 = mybir.dt.int32
AF = mybir.ActivationFunctionType
ALU = mybir.AluOpType
AX = mybir.AxisListType
ET = mybir.EngineType

S = 8192
D = 1024
NT = S // 128
GT = 2
GW = GT * 128
NG = NT // GT
ATT_W = 1
NKEY = (1 + ATT_W) * 128
NSLOT = GT + ATT_W
EPS = 1e-6
EPAD = 256
NBLK = 192
MPAD = NBLK * 128
C_Q, C_K, C_V, C_U, C_VS, C_GA, C_GB = 0, 512, 1024, 1536, 2048, 2560, 3584

SAME_ENGINE_SYNC = True
RAW_ONLY = True


class Buf:
    def __init__(self, name):
        self.name = name
        self.w = {}
        self.r = {}


class Sched:
    ENGS = ("pe", "act", "dve", "pool", "sp")

    def __init__(self, nc, es):
        self.nc = nc
        self.es = es
        self.ops = {e: [] for e in self.ENGS}
        self.sems = {}
        self.cnt = {}
        self.known = {e: {} for e in self.ENGS}
        for e in self.ENGS:
            self._sem("E_" + e)

    def _sem(self, key):
        if key not in self.sems:
            self.sems[key] = self.es.enter_context(self.nc.semaphore(key))
            self.cnt[key] = 0
        return key

    def _deps(self, eng, reads, writes, own_key):
        waits = {}

        def need(k, v, raw):
            if k == own_key:
                if eng == "pe" or not SAME_ENGINE_SYNC or (RAW_ONLY and not raw):
                    return
            if self.known[eng].get(k, 0) >= v:
                return
            waits[k] = max(waits.get(k, 0), v)

        for b in reads:
            for k, v in b.w.items():
                need(k, v, True)
        for b in writes:
            for k, v in b.w.items():
                need(k, v, False)
            for k, v in b.r.items():
                need(k, v, False)
        for k, v in waits.items():
            self.known[eng][k] = v
        return list(waits.items())

    def _commit(self, key, inc, reads, writes):
        self.cnt[key] += inc
        v = self.cnt[key]
        for b in reads:
            b.r[key] = max(b.r.get(key, 0), v)
        for b in writes:
            b.w[key] = max(b.w.get(key, 0), v)

    def op(self, eng, fn, reads=(), writes=()):
        key = "E_" + eng
        waits = self._deps(eng, reads, writes, key)
        self.ops[eng].append((waits, fn, key, 1))
        self._commit(key, 1, reads, writes)

    def dma(self, eng, fn, reads=(), writes=(), key="dma"):
        key = self._sem("D_" + key + ("_sw" if eng == "pool" else ""))
        waits = self._deps(eng, reads, writes, None)
        self.ops[eng].append((waits, fn, key, 16))
        self._commit(key, 16, reads, writes)

    def barrier(self, exclude=()):
        snap = {k: v for k, v in self.cnt.items() if k not in exclude}
        for e in self.ENGS:
            waits = []
            for k, v in snap.items():
                if v > 0 and self.known[e].get(k, 0) < v and k != "E_" + e:
                    waits.append((k, v))
                    self.known[e][k] = v
            if waits:
                self.ops[e].append((waits, None, None, 0))

    def _simulate(self):
        st = getattr(self, "_simstate", None)
        if st is None:
            st = self._simstate = {}
        pc = {e: 0 for e in self.ENGS}
        progress = True
        while progress:
            progress = False
            for e in self.ENGS:
                ops = self.ops[e]
                while pc[e] < len(ops):
                    waits, fn, key, inc = ops[pc[e]]
                    if any(st.get(k, 0) < v for k, v in waits):
                        break
                    if key is not None:
                        st[key] = st.get(key, 0) + inc
                    pc[e] += 1
                    progress = True
        stuck = {e: (pc[e], len(self.ops[e])) for e in self.ENGS if pc[e] < len(self.ops[e])}
        if stuck:
            msg = []
            for e, (p, n) in stuck.items():
                waits, fn, key, inc = self.ops[e][p]
                msg.append(f"{e}@{p}/{n} waits {[(k, v, st.get(k, 0)) for k, v in waits if st.get(k, 0) < v]} line {fn.__code__.co_firstlineno if fn else None}")
            raise RuntimeError("DEADLOCK in schedule: " + " | ".join(msg))

    def emit(self, final=False):
        self._simulate()
        with self.nc.Block() as block:
            def run(eng_name):
                ops = self.ops[eng_name]

                def body(eng):
                    for waits, fn, key, inc in ops:
                        for k, v in waits:
                            eng.wait_ge(self.sems[k], v)
                        if fn is not None:
                            ins = fn(eng)
                            ins.then_inc(self.sems[key], inc)
                    if final:
                        for k, v in self.cnt.items():
                            if v > 0 and k != "E_" + eng_name:
                                eng.wait_ge(self.sems[k], v)
                return body
            block.tensor(run("pe"))
            block.scalar(run("act"))
            block.vector(run("dve"))
            block.gpsimd(run("pool"))
            block.sync(run("sp"))
        self.ops = {e: [] for e in self.ENGS}


def sb_bcast(ap, shape_steps):
    return bass.AP(tensor=ap.tensor, offset=ap.offset, ap=[list(ap.ap[0])] + [list(s) for s in shape_steps])


def build_program(debug=False, n_groups=NG, do_moe=True, stop=None):
    nc = bass.Bass("TRN2", target_bir_lowering=False)
    dt_ = nc.dram_tensor
    kin = "ExternalInput"
    x_d = dt_("x", [S, D], F32, kind=kin)
    c_d = dt_("c2", [128, 8], F32, kind=kin)
    gmix_d = dt_("gmix2", [128, 8], F32, kind=kin)
    gffn_d = dt_("g_ffn", [1, D], F32, kind=kin)
    gfin_d = dt_("g_final", [1, D], F32, kind=kin)
    wada_d = dt_("w_ada", [D, 6 * D], F32, kind=kin)
    bada_d = dt_("b_ada", [1, 6 * D], F32, kind=kin)
    win_d = dt_("w_in", [D, 4608], F32, kind=kin)
    wsba_d = dt_("w_sba_out", [512, D], F32, kind=kin)
    gsgu_d = dt_("g_sgu", [1, 512], F32, kind=kin)
    wsp_d = dt_("w_spatial", [8, 128, 128], F32, kind=kin)
    bsp_d = dt_("b_spatial", [8, 128], F32, kind=kin)
    wsgu_d = dt_("w_sgu_out", [512, D], F32, kind=kin)
    wout_d = dt_("w_out", [D, D], F32, kind=kin)
    wr_d = dt_("w_r", [D, 36], F32, kind=kin)
    br_d = dt_("b_r", [1, 36], F32, kind=kin)
    weg_d = dt_("w_eg", [32, D, 512], F32, kind=kin)
    weu_d = dt_("w_eu", [32, D, 512], F32, kind=kin)
    wed_d = dt_("w_ed", [32, 512, D], F32, kind=kin)
    out_d = dt_("out", [S, D], F32, kind="ExternalOutput")
    dk = "ExternalOutput" if debug else "Internal"
    mod_d = dt_("mod_d", [1, 6 * D], F32, kind="Internal")
    h1_d = dt_("h1_d", [S, D], F32, kind=dk)
    n2_d = dt_("n2_d", [S + 1, D], BF16, kind="Internal")
    yb_d = dt_("yb_d", [MPAD, D], F32, kind="Internal")
    tok_d = dt_("tok_d", [MPAD, 1], I32, kind=dk)
    wegb_d = dt_("wegb_d", [32 * 128, 4096], BF16, kind="Internal")
    weub_d = dt_("weub_d", [32 * 128, 4096], BF16, kind="Internal")
    wedb_d = dt_("wedb_d", [32 * 128, 4096], BF16, kind="Internal")
    if debug:
        dbg_logits_d = dt_("dbg_logits", [128, NT * 36], F32, kind="ExternalOutput")
        dbg_dest_d = dt_("dbg_dest", [128, NT * 2], F32, kind="ExternalOutput")
        dbg_tw_d = dt_("dbg_tw", [128, NT * 2], F32, kind="ExternalOutput")
        dbg_blk_d = dt_("dbg_blk", [1, NBLK], I32, kind="ExternalOutput")

    ident_c = nc.inline_tensor(np.eye(128, dtype=np.float32), "ident_c")
    J_c = nc.inline_tensor(np.ascontiguousarray(np.eye(128, dtype=np.float32)[::-1]), "J_c")
    p_i = np.arange(128)[:, None]
    j_i = np.arange(NKEY)[None, :]
    mA = np.zeros((128, NKEY), np.float32)
    mA[:, :128] = (j_i[:, :128] <= 127 - p_i)
    mB = mA.copy()
    mB[:, 128:] = 1.0
    maskA_c = nc.inline_tensor(mA, "maskA_c")
    maskB_c = nc.inline_tensor(mB, "maskB_c")
    tril_c = nc.inline_tensor(np.tril(np.ones((128, 128), np.float32)), "tril_c")
    tris_c = nc.inline_tensor(np.triu(np.ones((32, 32), np.float32), 1), "tris_c")
    tokid_c = nc.inline_tensor((np.arange(NT)[None, :] * 128 + np.arange(128)[:, None]).astype(np.int32), "tokid_c")
    pidx_c = nc.inline_tensor(np.broadcast_to(np.arange(128, dtype=np.int32)[:, None], (128, NBLK)).copy(), "pidx_c")
    jrow_c = nc.inline_tensor(np.broadcast_to((np.arange(NBLK) * 128.0).astype(np.float32), (32, NBLK)).copy(), "jrow_c")

    es = ExitStack()
    with es:
        sc = Sched(nc, es)
        E = es.enter_context

        def sbt(name, shape, dtype):
            return E(nc.sbuf_tensor(name, shape, dtype))

        def pst(name, shape, dtype):
            return E(nc.psum_tensor(name, shape, dtype))

        ident_f = sbt("ident_f", [128, 128], F32)
        ident_b = sbt("ident_b", [128, 128], BF16)
        J_b = sbt("J_b", [128, 128], BF16)
        logits = sbt("logits", [128, NT, 36], F32)
        B_const = Buf("const")
        B_h1d = Buf("h1_d")
        B_n2d = Buf("n2_d")
        B_logits = Buf("logits")
        B_rows = Buf("rows")

        sc.dma("sp", lambda e: e.dma_start(out=ident_f[:], in_=ident_c.ap()), writes=[B_const], key="c_ident")
        J_f = sbt("J_f", [128, 128], F32)
        sc.dma("sp", lambda e: e.dma_start(out=J_f[:], in_=J_c.ap()), writes=[B_const], key="c_J")
        sc.op("dve", lambda e: e.tensor_copy(out=ident_b[:], in_=ident_f[:]), reads=[B_const], writes=[B_const])
        sc.op("dve", lambda e: e.tensor_copy(out=J_b[:], in_=J_f[:]), reads=[B_const], writes=[B_const])

        p1 = ExitStack()
        with p1:
            P = p1.enter_context

            def sb1(name, shape, dtype):
                return P(nc.sbuf_tensor(name, shape, dtype))

            def ps1(name, shape, dtype):
                return P(nc.psum_tensor(name, shape, dtype))

            win_sb = sb1("win_sb", [128, 8, 4608], BF16)
            wsba_sb = sb1("wsba_sb", [128, 4, D], BF16)
            wsgu_sb = sb1("wsgu_sb", [128, 4, D], BF16)
            wout_sb = sb1("wout_sb", [128, 8, D], BF16)
            wr_hi = sb1("wr_hi", [128, 8, 36], BF16)
            wr_lo = sb1("wr_lo", [128, 8, 36], BF16)
            br_row = sb1("br_row", [128, 36], F32)
            wct_sb = sb1("wct_sb", [128, 8, 128], BF16)
            bsb = sb1("bsb", [128, 4, 128], F32)
            gsgu_row = sb1("gsgu_row", [128, 512], F32)
            af_row = sb1("af_row", [128, D], F32)
            shf_row = sb1("shf_row", [128, D], F32)
            am_col = sb1("am_col", [128, 8], F32)
            shm_col = sb1("shm_col", [128, 8], F32)
            maskA = sb1("maskA", [128, NKEY], F32)
            B_w = Buf("weights")

            psP = [ps1(f"psP{i}", [128, 512], F32) for i in range(4)]
            psY = ps1("psY", [128, 512], F32)
            psTW = [ps1(f"psTW{i}", [128, 512], BF16) for i in range(2)]
            psT = [psTW[0][:, 0:GW], psTW[1][:, 0:GW]]
            psW = [psTW[0][:, GW:GW + NKEY], psTW[1][:, GW:GW + NKEY]]
            psZ_all = ps1("psZ_all", [128, 2 * NKEY], F32)
            psZ = [psZ_all[:, 0:NKEY], psZ_all[:, NKEY:2 * NKEY]]
            B_psP = [Buf(f"psP{i}") for i in range(4)]
            B_psY = Buf("psY")
            B_psT = [Buf(f"psT{i}") for i in range(2)]
            B_psZ = [Buf(f"psZ{i}") for i in range(2)]
            B_psW = [Buf(f"psW{i}") for i in range(2)]
            pp = [0]

            def next_psP():
                i = pp[0] % 4
                pp[0] += 1
                return psP[i], B_psP[i]

            for k in range(8):
                sc.dma("pool", lambda e, k=k: e.dma_start(out=win_sb[:, k, :], in_=win_d.ap()[k * 128:(k + 1) * 128, :]),
                       writes=[B_w], key="setup")
            for k in range(4):
                sc.dma("pool", lambda e, k=k: e.dma_start(out=wsba_sb[:, k, :], in_=wsba_d.ap()[k * 128:(k + 1) * 128, :]),
                       writes=[B_w], key="setup")
                sc.dma("pool", lambda e, k=k: e.dma_start(out=wsgu_sb[:, k, :], in_=wsgu_d.ap()[k * 128:(k + 1) * 128, :]),
                       writes=[B_w], key="setup")
            sc.dma("sp", lambda e: e.dma_start(out=br_row[:], in_=bass.AP(tensor=br_d.ap().tensor, offset=0, ap=[[0, 128], [1, 36]])),
                   writes=[B_w], key="setup")
            sc.dma("sp", lambda e: e.dma_start(out=gsgu_row[:], in_=bass.AP(tensor=gsgu_d.ap().tensor, offset=0, ap=[[0, 128], [1, 512]])),
                   writes=[B_w], key="setup")
            sc.dma("sp", lambda e: e.dma_start(out=maskA[:], in_=maskA_c.ap()), writes=[B_w], key="setup")
            for g in range(8):
                sc.dma("sp", lambda e, g=g: e.dma_start(
                    out=bsb[(g % 2) * 64:(g % 2) * 64 + 64, g // 2, :],
                    in_=bass.AP(tensor=bsp_d.ap().tensor, offset=g * 128, ap=[[0, 64], [1, 128]])),
                    writes=[B_w], key="setup")

            B_wexp = Buf("wexp")
            if do_moe:
                for e_ in range(32):
                    for src, dst in ((weg_d, wegb_d), (weu_d, weub_d), (wed_d, wedb_d)):
                        sc.dma("pool", lambda e, src=src, dst=dst, e_=e_: e.dma_start(
                            out=dst.ap()[e_ * 128:(e_ + 1) * 128, :], in_=src.ap()[e_].rearrange("(p k) n -> p (k n)", p=128)),
                            writes=[B_wexp], key="wconv")

            p0 = ExitStack()
            with p0:
                P0 = p0.enter_context
                c_sb = P0(nc.sbuf_tensor("c_sb", [128, 8], F32))
                cact = P0(nc.sbuf_tensor("cact", [128, 8], F32))
                wada_sb = [P0(nc.sbuf_tensor(f"wada_sb{i}", [128, 8, 256], F32)) for i in range(2)]
                bada_sb = [P0(nc.sbuf_tensor(f"bada_sb{i}", [1, 256], F32)) for i in range(2)]
                mod_sb = [P0(nc.sbuf_tensor(f"mod_sb{i}", [1, 256], F32)) for i in range(2)]
                modc = P0(nc.sbuf_tensor("modc", [128, 48], F32))
                gmix_sb = P0(nc.sbuf_tensor("gmix_sb", [128, 8], F32))
                gtm_row = P0(nc.sbuf_tensor("gtm_row", [128, D], F32))
                gffn_row = P0(nc.sbuf_tensor("gffn_row", [128, D], F32))
                scf_row = P0(nc.sbuf_tensor("scf_row", [128, D], F32))
                wout_f = [P0(nc.sbuf_tensor(f"wout_f{i}", [128, D], F32)) for i in range(2)]
                wsp_f = P0(nc.sbuf_tensor("wsp_f", [128, 8, 128], F32))
                wsp_b = P0(nc.sbuf_tensor("wsp_b", [128, 8, 128], BF16))
                tril_sb = P0(nc.sbuf_tensor("tril_sb", [128, 128], F32))
                zrow = P0(nc.sbuf_tensor("zrow", [1, D], BF16))
                wr_sb = P0(nc.sbuf_tensor("wr_sb", [128, 8, 36], F32))
                wr_t = P0(nc.sbuf_tensor("wr_t", [128, 8, 36], F32))
                B_wr = Buf("wr_sb")
                sc.dma("sp", lambda e: e.dma_start(out=wr_sb[:], in_=wr_d.ap().rearrange("(k p) n -> p k n", p=128)),
                       writes=[B_wr], key="c_wr")
                sc.op("dve", lambda e: e.tensor_copy(out=wr_hi[:], in_=wr_sb[:]), reads=[B_wr], writes=[B_w])
                sc.op("dve", lambda e: e.tensor_tensor(out=wr_t[:], in0=wr_sb[:], in1=wr_hi[:], op=ALU.subtract), reads=[B_wr, B_w], writes=[B_wr])
                sc.op("dve", lambda e: e.tensor_copy(out=wr_lo[:], in_=wr_t[:]), reads=[B_wr], writes=[B_w])
                B_c = Buf("c")
                B_wada = [Buf("wada0"), Buf("wada1")]
                B_bada = [Buf("bada0"), Buf("bada1")]
                B_mod = [Buf("mod_sb0"), Buf("mod_sb1")]
                B_modd = Buf("mod_d")
                B_modc = Buf("modc")
                B_p0 = Buf("p0misc")
                B_woutf = [Buf("woutf0"), Buf("woutf1")]

                sc.op("dve", lambda e: e.memset(zrow[:], 0.0), writes=[B_p0])
                sc.dma("sp", lambda e: e.dma_start(out=n2_d.ap()[S:S + 1, :], in_=zrow[:]), reads=[B_p0], writes=[B_n2d], key="zrow")
                sc.dma("sp", lambda e: e.dma_start(out=c_sb[:], in_=c_d.ap()), writes=[B_c], key="p0a_1")
                sc.dma("sp", lambda e: e.dma_start(out=gmix_sb[:], in_=gmix_d.ap()), writes=[B_c], key="p0a_2")
                sc.dma("sp", lambda e: e.dma_start(out=tril_sb[:], in_=tril_c.ap()), writes=[B_p0], key="p0a_3")
                sc.dma("sp", lambda e: e.dma_start(out=wsp_f[:], in_=wsp_d.ap().rearrange("g t s -> t g s")),
                       writes=[B_p0], key="p0a_4")
                sc.dma("sp", lambda e: e.dma_start(out=gffn_row[:], in_=bass.AP(tensor=gffn_d.ap().tensor, offset=0, ap=[[0, 128], [1, D]])),
                       writes=[B_p0], key="p0a_5")
                sc.op("act", lambda e: e.activation(out=cact[:], in_=c_sb[:], func=AF.Silu), reads=[B_c], writes=[B_c])
                for n in range(24):
                    sl = n % 2
                    sc.dma("sp", lambda e, n=n, sl=sl: e.dma_start(
                        out=wada_sb[sl][:], in_=wada_d.ap()[:, n * 256:(n + 1) * 256].rearrange("(k p) n -> p k n", p=128)),
                        writes=[B_wada[sl]], key=f"wada{sl}")
                    sc.dma("sp", lambda e, n=n, sl=sl: e.dma_start(out=bada_sb[sl][:], in_=bada_d.ap()[:, n * 256:(n + 1) * 256]),
                           writes=[B_bada[sl]], key=f"bada{sl}")
                    pt, bpt = next_psP()
                    for k in range(8):
                        sc.op("pe", lambda e, pt=pt, sl=sl, k=k: e.matmul(pt[0:1, 0:256], lhsT=cact[:, k:k + 1], rhs=wada_sb[sl][:, k, :],
                                                                        start=(k == 0), stop=(k == 7)),
                              reads=[B_c, B_wada[sl]], writes=[bpt])
                    sc.op("dve", lambda e, pt=pt, sl=sl: e.tensor_tensor(out=mod_sb[sl][:], in0=pt[0:1, 0:256], in1=bada_sb[sl][:], op=ALU.add),
                          reads=[bpt, B_bada[sl]], writes=[B_mod[sl]])
                    sc.dma("sp", lambda e, n=n, sl=sl: e.dma_start(out=mod_d.ap()[:, n * 256:(n + 1) * 256], in_=mod_sb[sl][:]),
                           reads=[B_mod[sl]], writes=[B_modd], key=f"p0b{sl}")
                sc.dma("sp", lambda e: e.dma_start(out=modc[:], in_=mod_d.ap().rearrange("o (j p) -> p (o j)", p=128),
                                                   allow_slow_non_contiguous=True),
                       reads=[B_modd], writes=[B_modc], key="p0c_6")

                def rowb(off):
                    return bass.AP(tensor=mod_d.ap().tensor, offset=off, ap=[[0, 128], [1, D]])
                sc.dma("sp", lambda e: e.dma_start(out=gtm_row[:], in_=rowb(2 * D)), reads=[B_modd], writes=[B_modc], key="p0c_7")
                sc.dma("sp", lambda e: e.dma_start(out=shf_row[:], in_=rowb(3 * D)), reads=[B_modd], writes=[B_modc], key="p0c_8")
                sc.dma("sp", lambda e: e.dma_start(out=scf_row[:], in_=rowb(4 * D)), reads=[B_modd], writes=[B_modc], key="p0c_9")
                sc.op("dve", lambda e: e.scalar_tensor_tensor(out=am_col[:], in0=modc[:, 8:16], scalar=1.0, in1=gmix_sb[:],
                                                              op0=ALU.add, op1=ALU.mult),
                      reads=[B_modc, B_c], writes=[B_w])
                sc.op("dve", lambda e: e.tensor_copy(out=shm_col[:], in_=modc[:, 0:8]), reads=[B_modc], writes=[B_w])
                sc.op("dve", lambda e: e.scalar_tensor_tensor(out=af_row[:], in0=scf_row[:], scalar=1.0, in1=gffn_row[:],
                                                              op0=ALU.add, op1=ALU.mult),
                      reads=[B_modc, B_p0], writes=[B_w])
                for k in range(8):
                    sl = k % 2
                    sc.dma("sp", lambda e, k=k, sl=sl: e.dma_start(out=wout_f[sl][:], in_=wout_d.ap()[k * 128:(k + 1) * 128, :]),
                           writes=[B_woutf[sl]], key=f"woutf{sl}")
                    sc.op("dve", lambda e, k=k, sl=sl: e.tensor_tensor(out=wout_sb[:, k, :], in0=wout_f[sl][:], in1=gtm_row[:], op=ALU.mult),
                          reads=[B_woutf[sl], B_modc], writes=[B_w])
                for g in range(8):
                    sc.op("dve", lambda e, g=g: e.tensor_tensor(out=wsp_b[:, g, :], in0=wsp_f[:, g, :], in1=tril_sb[:], op=ALU.mult),
                          reads=[B_p0], writes=[B_p0])
                for g in range(8):
                    h = g % 2
                    sc.op("pe", lambda e, g=g, h=h: e.transpose(psT[h][:, 0:128], wsp_b[:, g, :], ident_b[:]),
                          reads=[B_p0, B_const], writes=[B_psT[h]])
                    sc.op("act", lambda e, g=g, h=h: e.activation(out=wct_sb[:, g, :], in_=psT[h][:, 0:128], func=AF.Copy),
                          reads=[B_psT[h]], writes=[B_w])
                sc.barrier(exclude=("D_wconv_sw",))
                sc.emit(final=(stop == 'p0'))
                if stop == 'p0':
                    return nc

            xg2 = [sb1(f"xg{i}", [128, GT, D], F32) for i in range(2)]
            B_xg2 = [Buf("xg0"), Buf("xg1")]
            xs = sb1("xs", [128, GT, D], BF16)
            B_xs = Buf("xs")
            stat = sb1("stat", [128, 32], F32)
            B_ss = Buf("ss")
            B_rstd = Buf("rstd")
            nT = sb1("nT", [128, 8, GW], BF16)
            B_nT = Buf("nT")
            qT = sb1("qT", [128, 4, GW], BF16)
            B_qT = Buf("qT")
            uT = sb1("uT", [128, 4, GW], F32)
            B_uT = Buf("uT")
            k_tm = sb1("k_tm", [128, GT, 512], BF16)
            v_tm = sb1("v_tm", [128, GT, 512], BF16)
            vs = sb1("vs", [128, GT, 512], F32)
            B_ktm = Buf("k_tm")
            B_vtm = Buf("v_tm")
            B_vs = Buf("vs")
            kTr = sb1("kTr", [128, 4, NSLOT * 128], BF16)
            vr = sb1("vr", [128, NSLOT, 512], BF16)
            B_kTr = Buf("kTr")
            B_vr = Buf("vr")
            r_sb = [sb1(f"r_sb{i}", [128, NKEY], F32) for i in range(2)]
            Pb = [sb1(f"Pb{i}", [128, NKEY + 1], F32) for i in range(2)]
            w_bf = [sb1(f"w_bf{i}", [128, NKEY], BF16) for i in range(2)]
            wT = [sb1(f"wT{i}", [128, NKEY], BF16) for i in range(2)]
            B_r = [Buf("r0"), Buf("r1")]
            B_Pb = [Buf("Pb0"), Buf("Pb1")]
            B_wbf = [Buf("wbf0"), Buf("wbf1")]
            B_wT = [Buf("wT0"), Buf("wT1")]
            yaT = sb1("yaT", [128, 4, GW], BF16)
            ybT = sb1("ybT", [128, 4, GW], BF16)
            B_yaT = Buf("yaT")
            B_ybT = Buf("ybT")
            vn = sb1("vn", [128, 512], F32)
            vnb2 = sb1("vnb2", [128, GT, 512], BF16)
            B_vn = Buf("vn")
            B_vnb = Buf("vnb")
            bnst = sb1("bnst", [128, GT, 6], F32)
            mv = sb1("mv", [128, GT, 2], F32)
            B_mv = Buf("mv")
            tmpf = [sb1("tmpf0", [128, 512], F32), sb1("tmpf1", [128, GW], F32), sb1("tmpf2", [128, GW], F32)]
            B_tmpf = [Buf(f"tmpf{i}") for i in range(3)]
            maskB = tmpf[1]
            sc.dma("sp", lambda e: e.dma_start(out=maskB[:], in_=maskB_c.ap()), writes=[B_tmpf[1]], key="c_maskB")
            mT = sb1("mT", [128, 8, GW], BF16)
            B_mT = Buf("mT")
            n2f = sb1("n2f", [128, D], F32)
            B_n2f = Buf("n2f")
            n2b = sb1("n2b", [128, D], BF16)
            B_n2b = Buf("n2b")
            junk = n2b
            B_junk = B_n2b
            hiT = sb1("hiT", [128, 8, 128], BF16)
            loT = sb1("loT", [128, 8, 128], BF16)
            B_n2T = Buf("n2T")
            n2lo = tmpf[0][:].bitcast(BF16)

            for i in range(2):
                sc.op("dve", lambda e, i=i: e.memset(Pb[i][:, 0:1], 1.0), writes=[B_Pb[i]])
            sc.op("dve", lambda e: e.memset(kTr[:], 0.0), writes=[B_kTr])
            sc.op("dve", lambda e: e.memset(vr[:], 0.0), writes=[B_vr])

            def grp_ctx(G):
                return xg2[G % 2], B_xg2[G % 2]

            def g_load(G):
                xg, B_xg = grp_ctx(G)
                sc.dma("sp", lambda e, G=G: e.dma_start(out=xg[:], in_=x_d.ap()[G * GW:(G + 1) * GW, :].rearrange("(i p) d -> p i d", p=128)),
                       writes=[B_xg], key=f"xg{G % 2}")

            def g_front_a(G):
                X, BX = grp_ctx(G)
                sc.op("dve", lambda e: e.memset(stat[:, 0:4], 0.0), writes=[B_ss])
                for i in range(GT):
                    sc.op("act", lambda e, i=i: e.activation(out=junk[:], in_=X[:, i, :], func=AF.Square, accum_out=stat[:, i:i + 1]),
                          reads=[BX, B_ss], writes=[B_junk, B_ss])
                sc.op("act", lambda e: e.activation(out=stat[:, 4:4 + GT], in_=stat[:, 0:GT], func=AF.Sqrt, scale=1.0 / D, bias=EPS),
                      reads=[B_ss], writes=[B_ss])
                sc.op("dve", lambda e: e.reciprocal(out=stat[:, 8:8 + GT], in_=stat[:, 4:4 + GT]), reads=[B_ss], writes=[B_rstd])
                for i in range(GT):
                    sc.op("dve", lambda e, i=i: e.tensor_scalar(out=xs[:, i, :], in0=X[:, i, :], scalar1=stat[:, 8 + i:9 + i], scalar2=None,
                                                              op0=ALU.mult),
                          reads=[BX, B_rstd], writes=[B_xs])

            def g_front_b(G):
                X, BX = grp_ctx(G)
                for k in range(8):
                    tb = k % 2
                    for i in range(GT):
                        sc.op("pe", lambda e, k=k, i=i, tb=tb: e.transpose(psT[tb][:, i * 128:(i + 1) * 128], xs[:, i, k * 128:(k + 1) * 128], ident_b[:]),
                              reads=[B_xs, B_const], writes=[B_psT[tb]])
                    sc.op("act", lambda e, k=k, tb=tb: e.activation(out=nT[:, k, :], in_=psT[tb][:, 0:GW], func=AF.Identity,
                                                                  scale=am_col[:, k:k + 1], bias=shm_col[:, k:k + 1]),
                          reads=[B_psT[tb], B_w], writes=[B_nT])
                for m in range(4):
                    pt, bpt = next_psP()
                    for k in range(8):
                        sc.op("pe", lambda e, pt=pt, m=m, k=k: e.matmul(pt[:, 0:GW], lhsT=win_sb[:, k, C_Q + m * 128:C_Q + (m + 1) * 128], rhs=nT[:, k, :],
                                                                      start=(k == 0), stop=(k == 7)),
                              reads=[B_w, B_nT], writes=[bpt])
                    sc.op("act", lambda e, pt=pt, m=m: e.activation(out=qT[:, m, :], in_=pt[:, 0:GW], func=AF.Copy, scale=0.125),
                          reads=[bpt], writes=[B_qT])
                for i in range(GT):
                    for (col, dst, bdst) in ((C_K, k_tm, B_ktm), (C_V, v_tm, B_vtm)):
                        pt, bpt = next_psP()
                        for k in range(8):
                            sc.op("pe", lambda e, pt=pt, i=i, k=k, col=col: e.matmul(pt[:], lhsT=nT[:, k, i * 128:(i + 1) * 128],
                                                                                   rhs=win_sb[:, k, col:col + 512], start=(k == 0), stop=(k == 7)),
                                  reads=[B_w, B_nT], writes=[bpt])
                        sc.op("dve", lambda e, pt=pt, i=i, dst=dst: e.tensor_copy(out=dst[:, i, :], in_=pt[:]),
                              reads=[bpt], writes=[bdst])
                for i in range(GT):
                    pos = (GT - 1 - i)
                    pt, bpt = next_psP()
                    for c in range(4):
                        sc.op("pe", lambda e, pt=pt, i=i, c=c: e.matmul(pt[:, c * 128:(c + 1) * 128], lhsT=k_tm[:, i, c * 128:(c + 1) * 128], rhs=J_b[:],
                                                                      start=True, stop=True),
                              reads=[B_ktm, B_const], writes=[bpt])
                    sc.op("act", lambda e, pt=pt, pos=pos: e.activation(out=kTr[:, :, pos * 128:(pos + 1) * 128],
                                                                      in_=pt[:].rearrange("p (a b) -> p a b", a=4), func=AF.Copy),
                          reads=[bpt], writes=[B_kTr])
                    pt2, bpt2 = next_psP()
                    sc.op("pe", lambda e, pt2=pt2, i=i: e.matmul(pt2[:], lhsT=J_b[:], rhs=v_tm[:, i, :], start=True, stop=True),
                          reads=[B_vtm, B_const], writes=[bpt2])
                    sc.op("dve", lambda e, pt2=pt2, pos=pos: e.tensor_copy(out=vr[:, pos, :], in_=pt2[:]),
                          reads=[bpt2], writes=[B_vr])

                for i in range(GT):
                    pt, bpt = next_psP()
                    for k in range(8):
                        sc.op("pe", lambda e, pt=pt, i=i, k=k: e.matmul(pt[:], lhsT=nT[:, k, i * 128:(i + 1) * 128],
                                                                      rhs=win_sb[:, k, C_VS:C_VS + 512], start=(k == 0), stop=(k == 7)),
                              reads=[B_w, B_nT], writes=[bpt])
                    sc.op("act", lambda e, pt=pt, i=i: e.activation(out=vs[:, i, :], in_=pt[:], func=AF.Gelu_apprx_tanh),
                          reads=[bpt], writes=[B_vs])

            def g_back1a(G):
                X, BX = grp_ctx(G)
                for i in range(GT):
                    sc.op("dve", lambda e, i=i: e.bn_stats(out=bnst[:, i, :], in_=vs[:, i, :]), reads=[B_vs], writes=[B_mv])
                    sc.op("dve", lambda e, i=i: e.bn_aggr(out=mv[:, i, :], in_=bnst[:, i, :]), reads=[B_mv], writes=[B_mv])
                sc.op("act", lambda e: e.activation(out=stat[:, 12:12 + GT], in_=mv[:, :, 1], func=AF.Sqrt, scale=1.0, bias=EPS),
                      reads=[B_mv], writes=[B_ss])
                sc.op("dve", lambda e: e.reciprocal(out=stat[:, 16:16 + GT], in_=stat[:, 12:12 + GT]), reads=[B_ss], writes=[B_rstd])
                for i in range(GT):
                    sc.op("dve", lambda e, i=i: e.tensor_scalar(out=vn[:], in0=vs[:, i, :], scalar1=mv[:, i, 0:1], scalar2=stat[:, 16 + i:17 + i],
                                                              op0=ALU.subtract, op1=ALU.mult),
                          reads=[B_vs, B_mv, B_rstd], writes=[B_vn])
                    sc.op("dve", lambda e, i=i: e.tensor_tensor(out=vnb2[:, i, :], in0=vn[:], in1=gsgu_row[:], op=ALU.mult),
                          reads=[B_vn, B_w], writes=[B_vnb])
                hts = [(i, h) for i in range(GT) for h in (0, 2, 4, 6, 1, 3, 5, 7)]

                def att_A(n):
                    i, h = hts[n]
                    pos = GT - 1 - i
                    msk = maskB if (G == 0 and i == 0) else maskA
                    c = h // 2
                    ro = (h % 2) * 64
                    ab = n % 2
                    sc.op("pe", lambda e, ab=ab, c=c, ro=ro, i=i, pos=pos: e.matmul(
                        psZ[ab][:], lhsT=qT[ro:ro + 64, c, i * 128:(i + 1) * 128],
                        rhs=kTr[ro:ro + 64, c, pos * 128:pos * 128 + NKEY], start=True, stop=True),
                        reads=[B_qT, B_kTr], writes=[B_psZ[ab]])
                    sc.op("act", lambda e, ab=ab: e.activation(out=r_sb[ab][:], in_=psZ[ab][:], func=AF.Sigmoid, scale=-1.0),
                          reads=[B_psZ[ab]], writes=[B_r[ab]])
                    sc.op("dve", lambda e, ab=ab, msk=msk: e.tensor_tensor_scan(out=Pb[ab][:, 1:NKEY + 1], data0=r_sb[ab][:], data1=msk[:],
                                                                              initial=1.0, op0=ALU.mult, op1=ALU.max),
                          reads=[B_r[ab], B_w] + ([B_tmpf[1]] if msk is maskB else []), writes=[B_Pb[ab]])
                    sc.op("dve", lambda e, ab=ab: e.tensor_tensor(out=w_bf[ab][:], in0=Pb[ab][:, 0:NKEY], in1=Pb[ab][:, 1:NKEY + 1], op=ALU.subtract),
                          reads=[B_Pb[ab]], writes=[B_wbf[ab]])

                def att_B(n):
                    i, h = hts[n]
                    pos = GT - 1 - i
                    c = h // 2
                    ro = (h % 2) * 64
                    ab = n % 2
                    for j in range(1 + ATT_W):
                        sc.op("pe", lambda e, ab=ab, j=j: e.transpose(psW[ab][:, j * 128:(j + 1) * 128], w_bf[ab][:, j * 128:(j + 1) * 128], ident_b[:]),
                              reads=[B_wbf[ab], B_const], writes=[B_psW[ab]])
                    sc.op("act", lambda e, ab=ab: e.activation(out=wT[ab][:], in_=psW[ab][:], func=AF.Copy),
                          reads=[B_psW[ab]], writes=[B_wT[ab]])
                    for j in range(1 + ATT_W):
                        sc.op("pe", lambda e, ab=ab, j=j, c=c, ro=ro, pos=pos, h=h: e.matmul(
                            psY[ro:ro + 64, c * 128:(c + 1) * 128], lhsT=vr[:, pos + j, h * 64:(h + 1) * 64],
                            rhs=wT[ab][:, j * 128:(j + 1) * 128], start=(j == 0), stop=(j == ATT_W)),
                            reads=[B_vr, B_wT[ab]], writes=[B_psY])
                    if h == 7:
                        sc.op("act", lambda e, i=i: e.activation(out=yaT[:, :, i * 128:(i + 1) * 128], in_=psY[:].rearrange("p (a b) -> p a b", a=4), func=AF.Copy),
                              reads=[B_psY], writes=[B_yaT])

                att_A(0)
                for n in range(len(hts)):
                    if n + 1 < len(hts):
                        att_A(n + 1)
                    att_B(n)
                sc.op("act", lambda e: e.activation(out=kTr[:, :, GT * 128:(GT + ATT_W) * 128], in_=kTr[:, :, 0:ATT_W * 128], func=AF.Copy),
                      reads=[B_kTr], writes=[B_kTr])
                sc.op("dve", lambda e: e.tensor_copy(out=vr[:, GT:GT + ATT_W, :], in_=vr[:, 0:ATT_W, :]),
                      reads=[B_vr], writes=[B_vr])
                for m in range(4):
                    pt, bpt = next_psP()
                    for k in range(8):
                        sc.op("pe", lambda e, pt=pt, m=m, k=k: e.matmul(pt[:, 0:GW], lhsT=win_sb[:, k, C_U + m * 128:C_U + (m + 1) * 128], rhs=nT[:, k, :],
                                                                      start=(k == 0), stop=(k == 7)),
                              reads=[B_w, B_nT], writes=[bpt])
                    sc.op("act", lambda e, pt=pt, m=m: e.activation(out=uT[:, m, :], in_=pt[:, 0:GW], func=AF.Gelu_apprx_tanh),
                          reads=[bpt], writes=[B_uT])
                for i in range(GT):
                    pt, bpt = next_psP()
                    for g in range(8):
                        c = g // 2
                        ro = (g % 2) * 64
                        sc.op("pe", lambda e, pt=pt, g=g, c=c, ro=ro, i=i: e.matmul(pt[ro:ro + 64, c * 128:(c + 1) * 128], lhsT=vnb2[:, i, g * 64:(g + 1) * 64],
                                                                                  rhs=wct_sb[:, g, :], start=True, stop=True),
                              reads=[B_vnb, B_w], writes=[bpt])
                    sc.op("dve", lambda e, pt=pt: e.tensor_tensor(out=tmpf[0][:], in0=pt[:], in1=bsb[:].rearrange("p a b -> p (a b)"), op=ALU.add),
                          reads=[bpt, B_w], writes=[B_tmpf[0]])
                    sc.op("dve", lambda e, i=i: e.tensor_tensor(out=ybT[:, :, i * 128:(i + 1) * 128], in0=tmpf[0][:].rearrange("p (a b) -> p a b", a=4),
                                                              in1=uT[:, :, i * 128:(i + 1) * 128], op=ALU.mult),
                          reads=[B_tmpf[0], B_uT], writes=[B_ybT])

            def g_back1b(G):
                X, BX = grp_ctx(G)
                for m in range(8):
                    pAB, bA = next_psP()
                    pGG, bGa = next_psP()
                    bB = bA
                    bGb = bGa
                    pA, pB = pAB[:, 0:GW], pAB[:, GW:2 * GW]
                    pGa, pGb = pGG[:, 0:GW], pGG[:, GW:2 * GW]
                    for kc in range(4):
                        sc.op("pe", lambda e, pA=pA, kc=kc, m=m: e.matmul(pA[:], lhsT=wsba_sb[:, kc, m * 128:(m + 1) * 128], rhs=yaT[:, kc, :],
                                                                        start=(kc == 0), stop=(kc == 3)),
                              reads=[B_w, B_yaT], writes=[bA])
                    for kc in range(4):
                        sc.op("pe", lambda e, pB=pB, kc=kc, m=m: e.matmul(pB[:], lhsT=wsgu_sb[:, kc, m * 128:(m + 1) * 128], rhs=ybT[:, kc, :],
                                                                        start=(kc == 0), stop=(kc == 3)),
                              reads=[B_w, B_ybT], writes=[bB])
                    for k in range(8):
                        sc.op("pe", lambda e, pGa=pGa, k=k, m=m: e.matmul(pGa[:], lhsT=win_sb[:, k, C_GA + m * 128:C_GA + (m + 1) * 128], rhs=nT[:, k, :],
                                                                        start=(k == 0), stop=(k == 7)),
                              reads=[B_w, B_nT], writes=[bGa])
                    for k in range(8):
                        sc.op("pe", lambda e, pGb=pGb, k=k, m=m: e.matmul(pGb[:], lhsT=win_sb[:, k, C_GB + m * 128:C_GB + (m + 1) * 128], rhs=nT[:, k, :],
                                                                        start=(k == 0), stop=(k == 7)),
                              reads=[B_w, B_nT], writes=[bGb])
                    sc.op("act", lambda e, pGa=pGa: e.activation(out=tmpf[1][:, 0:GW], in_=pGa[:], func=AF.Sigmoid), reads=[bGa], writes=[B_tmpf[1]])
                    sc.op("act", lambda e, pGb=pGb: e.activation(out=tmpf[2][:, 0:GW], in_=pGb[:], func=AF.Sigmoid), reads=[bGb], writes=[B_tmpf[2]])
                    sc.op("dve", lambda e, pA=pA: e.tensor_tensor(out=tmpf[1][:, 0:GW], in0=tmpf[1][:, 0:GW], in1=pA[:], op=ALU.mult),
                          reads=[bA, B_tmpf[1]], writes=[B_tmpf[1]])
                    sc.op("dve", lambda e, pB=pB: e.tensor_tensor(out=tmpf[2][:, 0:GW], in0=tmpf[2][:, 0:GW], in1=pB[:], op=ALU.mult),
                          reads=[bB, B_tmpf[2]], writes=[B_tmpf[2]])
                    sc.op("dve", lambda e, m=m: e.tensor_tensor(out=mT[:, m, :], in0=tmpf[1][:, 0:GW], in1=tmpf[2][:, 0:GW], op=ALU.add),
                          reads=[B_tmpf[1], B_tmpf[2]], writes=[B_mT])

            def g_back2(G):
                X, BX = grp_ctx(G)
                sc.op("dve", lambda e: e.memset(stat[:, 20:24], 0.0), writes=[B_ss])
                def w1(i):
                    for half in range(2):
                        pt, bpt = next_psP()
                        for k in range(8):
                            sc.op("pe", lambda e, pt=pt, i=i, k=k, half=half: e.matmul(pt[:], lhsT=mT[:, k, i * 128:(i + 1) * 128],
                                                                                     rhs=wout_sb[:, k, half * 512:(half + 1) * 512],
                                                                                     start=(k == 0), stop=(k == 7)),
                                  reads=[B_w, B_mT], writes=[bpt])
                        sc.op("dve", lambda e, pt=pt, i=i, half=half: e.tensor_tensor(out=X[:, i, half * 512:(half + 1) * 512], in0=pt[:],
                                                                                    in1=X[:, i, half * 512:(half + 1) * 512], op=ALU.add),
                              reads=[bpt, BX], writes=[BX])
                    sc.op("act", lambda e, i=i: e.activation(out=vn[:].bitcast(BF16), in_=X[:, i, :], func=AF.Square, accum_out=stat[:, 20 + i:21 + i]),
                          reads=[BX, B_ss], writes=[B_vn, B_ss])


                def n2(i):
                    T = G * GT + i
                    sc.op("act", lambda e, i=i: e.activation(out=stat[:, 24 + i:25 + i], in_=stat[:, 20 + i:21 + i], func=AF.Sqrt, scale=1.0 / D, bias=EPS),
                          reads=[B_ss], writes=[B_ss])
                    sc.op("dve", lambda e, i=i: e.reciprocal(out=stat[:, 28 + i:29 + i], in_=stat[:, 24 + i:25 + i]), reads=[B_ss], writes=[B_rstd])
                    sc.op("dve", lambda e, i=i: e.scalar_tensor_tensor(out=n2f[:], in0=X[:, i, :], scalar=stat[:, 28 + i:29 + i], in1=af_row[:],
                                                                     op0=ALU.mult, op1=ALU.mult),
                          reads=[BX, B_rstd, B_w], writes=[B_n2f])
                    sc.op("dve", lambda e: e.tensor_tensor(out=n2f[:], in0=n2f[:], in1=shf_row[:], op=ALU.add),
                          reads=[B_n2f, B_w], writes=[B_n2f])
                    sc.op("act", lambda e: e.activation(out=n2b[:], in_=n2f[:], func=AF.Copy), reads=[B_n2f], writes=[B_n2b])
                    sc.dma("sp", lambda e, T=T: e.dma_start(out=n2_d.ap()[T * 128:(T + 1) * 128, :], in_=n2b[:]),
                           reads=[B_n2b], writes=[B_n2d], key="n2st")

                def router(i):
                    T = G * GT + i
                    sc.op("dve", lambda e: e.tensor_tensor(out=n2lo, in0=n2f[:], in1=n2b[:], op=ALU.subtract),
                          reads=[B_n2f, B_n2b], writes=[B_tmpf[0]])
                    for hlf in range(2):
                        for kk in range(4):
                            k = hlf * 4 + kk
                            sc.op("pe", lambda e, hlf=hlf, kk=kk, k=k: e.transpose(psTW[hlf][:, kk * 128:(kk + 1) * 128],
                                                                                 n2b[:, k * 128:(k + 1) * 128], ident_b[:]),
                                  reads=[B_n2b, B_const], writes=[B_psT[hlf], B_psW[hlf]])
                    lo_ps = []
                    for hlf in range(2):
                        ptl, bptl = next_psP()
                        ptl16 = ptl[:].bitcast(BF16)
                        lo_ps.append((ptl16, bptl))
                        for kk in range(4):
                            k = hlf * 4 + kk
                            sc.op("pe", lambda e, ptl16=ptl16, kk=kk, k=k: e.transpose(ptl16[:, kk * 128:(kk + 1) * 128],
                                                                                     n2lo[:, k * 128:(k + 1) * 128], ident_b[:]),
                                  reads=[B_tmpf[0], B_const], writes=[bptl])
                    sc.op("act", lambda e: e.activation(out=hiT[:, 0:4, :], in_=psTW[0][:].rearrange("p (a b) -> p a b", a=4), func=AF.Copy),
                          reads=[B_psT[0], B_psW[0]], writes=[B_n2T])
                    sc.op("dve", lambda e: e.tensor_copy(out=hiT[:, 4:8, :], in_=psTW[1][:].rearrange("p (a b) -> p a b", a=4)),
                          reads=[B_psT[1], B_psW[1]], writes=[B_n2T])
                    sc.op("act", lambda e, p16=lo_ps[0][0]: e.activation(out=loT[:, 0:4, :], in_=p16[:, 0:512].rearrange("p (a b) -> p a b", a=4), func=AF.Copy),
                          reads=[lo_ps[0][1]], writes=[B_n2T])
                    sc.op("dve", lambda e, p16=lo_ps[1][0]: e.tensor_copy(out=loT[:, 4:8, :], in_=p16[:, 0:512].rearrange("p (a b) -> p a b", a=4)),
                          reads=[lo_ps[1][1]], writes=[B_n2T])
                    pt, bpt = next_psP()
                    terms = [(hiT, wr_hi), (hiT, wr_lo), (loT, wr_hi)]
                    for ti, (aT, wv) in enumerate(terms):
                        for k in range(8):
                            sc.op("pe", lambda e, pt=pt, k=k, aT=aT, wv=wv, ti=ti: e.matmul(pt[:, 0:36], lhsT=aT[:, k, :], rhs=wv[:, k, :],
                                                                                         start=(ti == 0 and k == 0), stop=(ti == 2 and k == 7)),
                                  reads=[B_n2T, B_w], writes=[bpt])
                    sc.op("dve", lambda e, pt=pt, T=T: e.tensor_tensor(out=logits[:, T, :], in0=pt[:, 0:36], in1=br_row[:], op=ALU.add),
                          reads=[bpt, B_w], writes=[B_logits])


                assert GT == 2
                w1(0)
                n2(0)
                w1(1)
                sc.dma("sp", lambda e, G=G: e.dma_start(out=h1_d.ap()[G * GW:(G + 1) * GW, :].rearrange("(i p) d -> p i d", p=128), in_=X[:]),
                       reads=[BX], writes=[B_h1d], key="h1st")
                router(0)
                n2(1)
                router(1)

            g_load(0)
            g_front_a(0)
            g_front_b(0)
            for G in range(n_groups):
                if G + 1 < n_groups:
                    g_load(G + 1)
                g_back1a(G)
                if G + 1 < n_groups:
                    g_front_a(G + 1)
                g_back1b(G)
                if G + 1 < n_groups:
                    g_front_b(G + 1)
                g_back2(G)
            if debug:
                for nm, t_, bb, shp, dt2 in (("nT", nT, B_nT, [128, 8 * GW], BF16), ("qT", qT, B_qT, [128, 4 * GW], BF16),
                                             ("uT", uT, B_uT, [128, 4 * GW], F32), ("yaT", yaT, B_yaT, [128, 4 * GW], BF16),
                                             ("ybT", ybT, B_ybT, [128, 4 * GW], BF16), ("mT", mT, B_mT, [128, 8 * GW], BF16),
                                             ("kTr", kTr, B_kTr, [128, 4 * NSLOT * 128], BF16), ("vr", vr, B_vr, [128, NSLOT * 512], BF16),
                                             ("vs", vs, B_vs, [128, GT * 512], F32), ("amc", am_col, B_w, [128, 8], F32),
                                             ("woutb", wout_sb, B_w, [128, 8 * D], BF16),
                                             ("ktm", k_tm, B_ktm, [128, GT * 512], BF16), ("vtm", v_tm, B_vtm, [128, GT * 512], BF16),
                                             ("Jb", J_b, B_const, [128, 128], BF16)):
                    dd = nc.dram_tensor("dbg_" + nm, shp, dt2, kind="ExternalOutput")
                    src = t_[:].rearrange("p a b -> p (a b)") if len(t_.shape) == 3 else t_[:]
                    sc.dma("sp", lambda e, dd=dd, src=src: e.dma_start(out=dd.ap(), in_=src), reads=[bb], key="dbg")
            sc.barrier(exclude=("D_wconv_sw",))
            sc.emit(final=(stop == 'p1'))
            if stop == 'p1':
                return nc

        if debug:
            sc.dma("sp", lambda e: e.dma_start(out=dbg_logits_d.ap(), in_=logits[:].rearrange("p a b -> p (a b)")),
                   reads=[B_logits], key="dbg")

        if do_moe:
            _moe_phases(nc, sc, locals(), stop)
        else:
            _final_only(nc, sc, locals())

    return nc


def _final_only(nc, sc, L):
    raise NotImplementedError


def _moe_phases(nc, sc, L, stop=None):
    logits = L["logits"]; B_logits = L["B_logits"]; B_const = L["B_const"]; B_rows = L["B_rows"]
    ident_f = L["ident_f"]; ident_b = L["ident_b"]
    mod_d = L["mod_d"]; gfin_d = L["gfin_d"]
    h1_d = L["h1_d"]; n2_d = L["n2_d"]; yb_d = L["yb_d"]; tok_d = L["tok_d"]; out_d = L["out_d"]
    wegb_d = L["wegb_d"]; weub_d = L["weub_d"]; wedb_d = L["wedb_d"]
    B_wexp = L["B_wexp"]; B_h1d = L["B_h1d"]; B_n2d = L["B_n2d"]
    tris_c = L["tris_c"]; tokid_c = L["tokid_c"]; jrow_c = L["jrow_c"]; pidx_c = L["pidx_c"]
    debug = L["debug"]

    es2 = ExitStack()
    with es2:
        P = es2.enter_context
        dest_i = P(nc.sbuf_tensor("dest_i", [128, NT, 2], I32))
        tw = P(nc.sbuf_tensor("tw", [128, NT, 2], F32))
        tokidx = P(nc.sbuf_tensor("tokidx", [128, NBLK], I32))
        blk_i = P(nc.sbuf_tensor("blk_i", [1, NBLK], I32))
        widx = P(nc.sbuf_tensor("widx", [128, NBLK], I32))
        B_dest = Buf("dest_i"); B_tw = Buf("tw"); B_tokidx = Buf("tokidx"); B_blk = Buf("blk_i")
        B_tokd = Buf("tok_d")

        p2 = ExitStack()
        with p2:
            Q = p2.enter_context

            def sb2(name, shape, dtype):
                return Q(nc.sbuf_tensor(name, shape, dtype))
            gmax = sb2("gmax", [128, NT], F32)
            goh = sb2("goh", [128, NT, 4], F32)
            gsh = sb2("gsh", [128, NT, 4], F32)
            gsum = sb2("gsum", [128, NT], F32)
            gw = sb2("gw", [128, NT], F32)
            el = sb2("el", [128, NT, 8], F32)
            el2 = sb2("el2", [128, NT, 8], F32)
            elt = sb2("elt", [128, NT, 8], F32)
            m1 = sb2("m1", [128, NT], F32)
            m2 = sb2("m2", [128, NT], F32)
            oh1 = sb2("oh1", [128, NT, 8], F32)
            oh2 = sb2("oh2", [128, NT, 8], F32)
            dm = sb2("dm", [128, NT], F32)
            w1 = sb2("w1", [128, NT], F32)
            A1 = sb2("A1", [128, NT, 32], F32)
            A2 = sb2("A2", [128, NT, 32], F32)
            A1T = sb2("A1T", [32, S], F32)
            A2T = sb2("A2T", [32, S], F32)
            AT = sb2("AT", [32, S], F32)
            CT = sb2("CT", [32, S], F32)
            sm = sb2("sm", [32, 16], F32)
            tris = sb2("tris", [32, 32], F32)
            ones32 = sb2("ones32", [32, 1], F32)
            jrow = sb2("jrow", [32, NBLK], F32)
            cmpb = sb2("cmpb", [32, NBLK], F32)
            blk_f = sb2("blk_f", [1, NBLK], F32)
            dest_f = sb2("dest_f", [128, NT, 2], F32)
            dmod = sb2("dmod", [128, NT, 2], F32)
            off_f = sb2("off_f", [128, NT, 2], F32)
            off_i = sb2("off_i", [128, NT, 2], I32)
            dmod_i = sb2("dmod_i", [128, NT, 2], I32)
            djv_i = sb2("djv_i", [128, NT, 2], I32)
            smi = sb2("smi", [32, 4], I32)
            tokid = sb2("tokid", [128, NT], I32)
            padidx = sb2("padidx", [128, NBLK], I32)
            ones1 = sb2("ones1", [1, 128], F32)
            eq_f = sb2("eq_f", [1, NBLK], F32)
            widx_f = sb2("widx_f", [128, NBLK], F32)
            pidx_f = sb2("pidx_f", [128, NBLK], F32)
            pidx_i = sb2("pidx_i", [128, NBLK], I32)
            psA_ = [Q(nc.psum_tensor(f"psA2_{i}", [128, 512], F32)) for i in range(2)]
            psA2_ = [Q(nc.psum_tensor(f"psB2_{i}", [128, 512], F32)) for i in range(2)]
            psS = Q(nc.psum_tensor("psS", [128, 512], F32))
            B = {n: Buf(n) for n in ("g", "el", "oh", "A", "AT", "sm", "c2", "dest", "ps0", "ps1", "pb0", "pb1", "psS", "off")}

            def bc3(t, inner, n_inner):
                a = t
                return bass.AP(tensor=a.tensor, offset=a.offset, ap=[list(a.ap[0]), list(a.ap[1]), [0, n_inner]])

            sc.dma("sp", lambda e: e.dma_start(out=tris[:], in_=tris_c.ap()), writes=[B["c2"]], key="p2c_11")
            sc.dma("sp", lambda e: e.dma_start(out=jrow[:], in_=jrow_c.ap()), writes=[B["c2"]], key="p2c_12")
            sc.dma("sp", lambda e: e.dma_start(out=tokid[:], in_=tokid_c.ap()), writes=[B["c2"]], key="p2c_13")
            sc.dma("sp", lambda e: e.dma_start(out=pidx_i[:], in_=pidx_c.ap()), writes=[B["c2"]], key="p2c_14")
            sc.op("dve", lambda e: e.tensor_copy(out=pidx_f[:], in_=pidx_i[:]), reads=[B["c2"]], writes=[B["c2"]])
            sc.op("dve", lambda e: e.memset(ones32[:], 1.0), writes=[B["c2"]])
            sc.op("dve", lambda e: e.memset(padidx[:], S), writes=[B["c2"]])
            sc.dma("sp", lambda e: e.dma_start(out=tok_d.ap().rearrange("(p j) o -> p (j o)", p=128), in_=padidx[:]),
                   reads=[B["c2"]], writes=[B_tokd], key="p2c_15")

            gl = logits[:, :, 0:4]
            sc.op("dve", lambda e: e.tensor_reduce(out=gmax[:], in_=gl, axis=AX.X, op=ALU.max), reads=[B_logits], writes=[B["g"]])
            sc.op("dve", lambda e: e.tensor_tensor(out=goh[:], in0=gl, in1=bc3(gmax[:], 0, 4), op=ALU.is_ge), reads=[B_logits, B["g"]], writes=[B["g"]])
            sc.op("dve", lambda e: e.tensor_tensor(out=gsh[:], in0=gl, in1=bc3(gmax[:], 0, 4), op=ALU.subtract), reads=[B_logits, B["g"]], writes=[B["g"]])
            sc.op("act", lambda e: e.activation(out=gsh[:], in_=gsh[:], func=AF.Exp), reads=[B["g"]], writes=[B["g"]])
            sc.op("dve", lambda e: e.tensor_reduce(out=gsum[:], in_=gsh[:], axis=AX.X, op=ALU.add), reads=[B["g"]], writes=[B["g"]])
            sc.op("dve", lambda e: e.reciprocal(out=gw[:], in_=gsum[:]), reads=[B["g"]], writes=[B["g"]])
            for g in range(4):
                src = logits[:, :, 4 + 8 * g:12 + 8 * g]
                gsel = goh[:, :, g]
                if g == 0:
                    sc.op("dve", lambda e, src=src, gsel=gsel: e.tensor_tensor(out=el[:], in0=src, in1=bc3(gsel, 0, 8), op=ALU.mult),
                          reads=[B_logits, B["g"]], writes=[B["el"]])
                else:
                    sc.op("dve", lambda e, src=src, gsel=gsel: e.tensor_tensor(out=elt[:], in0=src, in1=bc3(gsel, 0, 8), op=ALU.mult),
                          reads=[B_logits, B["g"]], writes=[B["el"]])
                    sc.op("dve", lambda e: e.tensor_tensor(out=el[:], in0=el[:], in1=elt[:], op=ALU.add), reads=[B["el"]], writes=[B["el"]])
            sc.op("dve", lambda e: e.tensor_reduce(out=m1[:], in_=el[:], axis=AX.X, op=ALU.max), reads=[B["el"]], writes=[B["oh"]])
            sc.op("dve", lambda e: e.tensor_tensor(out=oh1[:], in0=el[:], in1=bc3(m1[:], 0, 8), op=ALU.is_ge), reads=[B["el"], B["oh"]], writes=[B["oh"]])
            sc.op("dve", lambda e: e.scalar_tensor_tensor(out=el2[:], in0=oh1[:], scalar=-1e30, in1=el[:], op0=ALU.mult, op1=ALU.add),
                  reads=[B["el"], B["oh"]], writes=[B["oh"]])
            sc.op("dve", lambda e: e.tensor_reduce(out=m2[:], in_=el2[:], axis=AX.X, op=ALU.max), reads=[B["oh"]], writes=[B["oh"]])
            sc.op("dve", lambda e: e.tensor_tensor(out=oh2[:], in0=el2[:], in1=bc3(m2[:], 0, 8), op=ALU.is_ge), reads=[B["oh"]], writes=[B["oh"]])
            sc.op("dve", lambda e: e.tensor_tensor(out=dm[:], in0=m1[:], in1=m2[:], op=ALU.subtract), reads=[B["oh"]], writes=[B["oh"]])
            sc.op("act", lambda e: e.activation(out=w1[:], in_=dm[:], func=AF.Sigmoid), reads=[B["oh"]], writes=[B["oh"]])
            sc.op("dve", lambda e: e.tensor_tensor(out=tw[:, :, 0], in0=w1[:], in1=gw[:], op=ALU.mult), reads=[B["oh"], B["g"]], writes=[B_tw])
            sc.op("dve", lambda e: e.tensor_tensor(out=tw[:, :, 1], in0=gw[:], in1=tw[:, :, 0], op=ALU.subtract), reads=[B["g"], B_tw], writes=[B_tw])
            for g in range(4):
                gsel = goh[:, :, g]
                sc.op("dve", lambda e, g=g, gsel=gsel: e.tensor_tensor(out=A1[:, :, g * 8:(g + 1) * 8], in0=oh1[:], in1=bc3(gsel, 0, 8), op=ALU.mult),
                      reads=[B["oh"], B["g"]], writes=[B["A"]])
                sc.op("dve", lambda e, g=g, gsel=gsel: e.tensor_tensor(out=A2[:, :, g * 8:(g + 1) * 8], in0=oh2[:], in1=bc3(gsel, 0, 8), op=ALU.mult),
                      reads=[B["oh"], B["g"]], writes=[B["A"]])
            for r in range(NT // 4):
                for (Asrc, Adst, pss, bn) in ((A1, A1T, psA_, "ps"), (A2, A2T, psA2_, "pb")):
                    pb_ = r % 2
                    pt = pss[pb_]
                    bpt = B[f"{bn}{pb_}"]
                    for q in range(4):
                        T = r * 4 + q
                        sc.op("pe", lambda e, pt=pt, q=q, T=T, Asrc=Asrc: e.transpose(pt[0:32, q * 128:(q + 1) * 128], Asrc[:, T, :], ident_f[:]),
                              reads=[B["A"], B_const], writes=[bpt])
                    sc.op("act", lambda e, pt=pt, r=r, Adst=Adst: e.activation(out=Adst[:, r * 512:(r + 1) * 512], in_=pt[0:32, :], func=AF.Copy),
                          reads=[bpt], writes=[B["AT"]])
            sc.op("dve", lambda e: e.tensor_tensor(out=AT[:], in0=A1T[:], in1=A2T[:], op=ALU.add), reads=[B["AT"]], writes=[B["AT"]])
            sc.op("dve", lambda e: e.tensor_tensor_scan(out=CT[:], data0=AT[:], data1=AT[:], initial=0.0, op0=ALU.add, op1=ALU.max),
                  reads=[B["AT"]], writes=[B["AT"]])
            sc.op("dve", lambda e: e.tensor_copy(out=sm[:, 0:1], in_=CT[:, S - 1:S]), reads=[B["AT"]], writes=[B["sm"]])
            sc.op("dve", lambda e: e.tensor_scalar(out=sm[:, 1:2], in0=sm[:, 0:1], scalar1=float(EPAD - 1), scalar2=None, op0=ALU.add), reads=[B["sm"]], writes=[B["sm"]])
            sc.op("dve", lambda e: e.tensor_copy(out=smi[:, 0:1], in_=sm[:, 1:2]), reads=[B["sm"]], writes=[B["sm"]])
            sc.op("dve", lambda e: e.tensor_single_scalar(out=smi[:, 1:2], in_=smi[:, 0:1], scalar=8, op=ALU.arith_shift_right), reads=[B["sm"]], writes=[B["sm"]])
            sc.op("dve", lambda e: e.tensor_copy(out=sm[:, 2:3], in_=smi[:, 1:2]), reads=[B["sm"]], writes=[B["sm"]])
            sc.op("dve", lambda e: e.tensor_scalar(out=sm[:, 3:4], in0=sm[:, 2:3], scalar1=float(EPAD), scalar2=None, op0=ALU.mult), reads=[B["sm"]], writes=[B["sm"]])
            sc.op("pe", lambda e: e.matmul(psS[0:32, 0:1], lhsT=tris[:], rhs=sm[:, 3:4], start=True, stop=True), reads=[B["sm"], B["c2"]], writes=[B["psS"]])
            sc.op("dve", lambda e: e.tensor_copy(out=sm[:, 4:5], in_=psS[0:32, 0:1]), reads=[B["psS"]], writes=[B["sm"]])
            sc.op("dve", lambda e: e.tensor_tensor(out=sm[:, 5:6], in0=sm[:, 4:5], in1=sm[:, 3:4], op=ALU.add), reads=[B["sm"]], writes=[B["sm"]])
            sc.op("dve", lambda e: e.tensor_scalar(out=cmpb[:], in0=jrow[:], scalar1=sm[:, 5:6], scalar2=None, op0=ALU.is_ge),
                  reads=[B["sm"], B["c2"]], writes=[B["sm"]])
            sc.op("pe", lambda e: e.matmul(psS[0:1, 128:128 + NBLK], lhsT=ones32[:], rhs=cmpb[:], start=True, stop=True),
                  reads=[B["sm"], B["c2"]], writes=[B["psS"]])
            sc.op("dve", lambda e: e.tensor_scalar(out=blk_f[:], in0=psS[0:1, 128:128 + NBLK], scalar1=31.0, scalar2=None, op0=ALU.min),
                  reads=[B["psS"]], writes=[B["sm"]])
            sc.op("dve", lambda e: e.tensor_copy(out=blk_i[:], in_=blk_f[:]), reads=[B["sm"]], writes=[B_blk])
            sc.op("dve", lambda e: e.memset(ones1[:], 1.0), writes=[B["c2"]])
            sc.op("dve", lambda e: e.memset(eq_f[:], 0.0), writes=[B["sm"]])
            sc.op("dve", lambda e: e.tensor_tensor(out=eq_f[0:1, 2:NBLK], in0=blk_f[0:1, 2:NBLK], in1=blk_f[0:1, 0:NBLK - 2], op=ALU.is_equal),
                  reads=[B["sm"]], writes=[B["sm"]])
            sc.op("pe", lambda e: e.matmul(psA_[0][:, 0:NBLK], lhsT=ones1[:], rhs=blk_f[:], start=True, stop=True),
                  reads=[B["sm"], B["c2"]], writes=[B["ps0"]])
            sc.op("pe", lambda e: e.matmul(psA_[0][:, 256:256 + NBLK], lhsT=ones1[:], rhs=eq_f[:], start=True, stop=True),
                  reads=[B["sm"], B["c2"]], writes=[B["ps0"]])
            sc.op("dve", lambda e: e.scalar_tensor_tensor(out=widx_f[:], in0=psA_[0][:, 0:NBLK], scalar=128.0, in1=pidx_f[:],
                                                          op0=ALU.mult, op1=ALU.add),
                  reads=[B["ps0"], B["c2"]], writes=[B["sm"]])
            sc.op("dve", lambda e: e.scalar_tensor_tensor(out=widx_f[:], in0=psA_[0][:, 256:256 + NBLK], scalar=0.0, in1=widx_f[:],
                                                          op0=ALU.mult, op1=ALU.add),
                  reads=[B["ps0"], B["sm"]], writes=[B["sm"]])
            sc.op("dve", lambda e: e.tensor_copy(out=widx[:], in_=widx_f[:]), reads=[B["sm"]], writes=[B_blk])
            sc.op("dve", lambda e: e.tensor_tensor(out=CT[:], in0=CT[:], in1=AT[:], op=ALU.subtract), reads=[B["AT"]], writes=[B["AT"]])
            sc.op("dve", lambda e: e.tensor_scalar(out=CT[:], in0=CT[:], scalar1=sm[:, 4:5], scalar2=None, op0=ALU.add),
                  reads=[B["AT"], B["sm"]], writes=[B["AT"]])
            sc.op("dve", lambda e: e.tensor_tensor(out=A1T[:], in0=A1T[:], in1=CT[:], op=ALU.mult), reads=[B["AT"]], writes=[B["AT"]])
            sc.op("dve", lambda e: e.tensor_tensor(out=A2T[:], in0=A2T[:], in1=CT[:], op=ALU.mult), reads=[B["AT"]], writes=[B["AT"]])
            for T in range(NT):
                for k_, Dk in ((0, A1T), (1, A2T)):
                    col = 384 + T * 2 + k_
                    sc.op("pe", lambda e, T=T, Dk=Dk, col=col: e.matmul(psS[:, col:col + 1], lhsT=Dk[:, T * 128:(T + 1) * 128], rhs=ones32[:],
                                                                       start=True, stop=True),
                          reads=[B["AT"], B["c2"]], writes=[B["psS"]])
            sc.op("dve", lambda e: e.tensor_copy(out=dest_f[:].rearrange("p a b -> p (a b)"), in_=psS[:, 384:384 + 2 * NT]),
                  reads=[B["psS"]], writes=[B["dest"]])
            sc.op("dve", lambda e: e.tensor_copy(out=dest_i[:], in_=dest_f[:]), reads=[B["dest"]], writes=[B_dest])
            sc.op("dve", lambda e: e.tensor_single_scalar(out=dmod_i[:], in_=dest_i[:], scalar=127, op=ALU.bitwise_and), reads=[B_dest], writes=[B["off"]])
            sc.op("dve", lambda e: e.tensor_single_scalar(out=djv_i[:], in_=dest_i[:], scalar=7, op=ALU.arith_shift_right), reads=[B_dest], writes=[B["off"]])
            sc.op("dve", lambda e: e.tensor_copy(out=dmod[:], in_=dmod_i[:]), reads=[B["off"]], writes=[B["off"]])
            sc.op("dve", lambda e: e.tensor_copy(out=off_f[:], in_=djv_i[:]), reads=[B["off"]], writes=[B["off"]])
            sc.op("dve", lambda e: e.scalar_tensor_tensor(out=off_f[:], in0=dmod[:], scalar=float(NBLK), in1=off_f[:], op0=ALU.mult, op1=ALU.add),
                  reads=[B["off"]], writes=[B["off"]])
            sc.op("dve", lambda e: e.tensor_copy(out=off_i[:], in_=off_f[:]), reads=[B["off"]], writes=[B["off"]])
            for T in range(NT):
                for k_ in range(2):
                    sc.dma("pool", lambda e, T=T, k_=k_: e.indirect_dma_start(
                        out=tok_d.ap(), out_offset=bass.IndirectOffsetOnAxis(ap=off_i[:, T, k_:k_ + 1], axis=0),
                        in_=tokid[:, T:T + 1], in_offset=None),
                        reads=[B["off"], B["c2"]], writes=[B_tokd], key="scat")
            sc.dma("sp", lambda e: e.dma_start(out=tokidx[:], in_=tok_d.ap().rearrange("(p j) o -> p (j o)", p=128)),
                   reads=[B_tokd], writes=[B_tokidx], key="tokidx")
            if debug:
                sc.dma("sp", lambda e: e.dma_start(out=L["dbg_dest_d"].ap(), in_=dest_f[:].rearrange("p a b -> p (a b)")), reads=[B["dest"]], key="dbg")
                sc.dma("sp", lambda e: e.dma_start(out=L["dbg_tw_d"].ap(), in_=tw[:].rearrange("p a b -> p (a b)")), reads=[B_tw], key="dbg")
                sc.dma("sp", lambda e: e.dma_start(out=L["dbg_blk_d"].ap(), in_=blk_i[:]), reads=[B_blk], key="dbg")
            sc.barrier()
            sc.emit(final=(stop == 'p2'))
            if stop == 'p2':
                return

        p3 = ExitStack()
        with p3:
            Q = p3.enter_context
            wg = [Q(nc.sbuf_tensor(f"wg{i}", [128, 8, 512], BF16)) for i in range(3)]
            wu = [Q(nc.sbuf_tensor(f"wu{i}", [128, 8, 512], BF16)) for i in range(3)]
            wd = [Q(nc.sbuf_tensor(f"wd{i}", [128, 4, D], BF16)) for i in range(3)]
            xgat = [Q(nc.sbuf_tensor(f"xgat{i}", [128, D], BF16)) for i in range(2)]
            nTb = [Q(nc.sbuf_tensor(f"nTb{i}", [128, 8, 128], BF16)) for i in range(2)]
            slu = Q(nc.sbuf_tensor("slu", [128, 512], F32))
            hidT = [Q(nc.sbuf_tensor(f"hidT{i}", [128, 4, 128], BF16)) for i in range(2)]
            yo = [Q(nc.sbuf_tensor(f"yo{i}", [128, D], F32)) for i in range(2)]
            psXa = Q(nc.psum_tensor("psXa", [128, 512], BF16))
            psXb = Q(nc.psum_tensor("psXb", [128, 512], BF16))
            psG = [Q(nc.psum_tensor(f"psG{i}", [128, 512], F32)) for i in range(2)]
            psU = [Q(nc.psum_tensor(f"psU{i}", [128, 512], F32)) for i in range(2)]
            psO = [Q(nc.psum_tensor(f"psO{i}", [128, 512], F32)) for i in range(2)]
            B_wg = [Buf("wg0"), Buf("wg1"), Buf("wg2")]; B_xgat = [Buf("xgat0"), Buf("xgat1")]; B_nTb = [Buf("nTb0"), Buf("nTb1")]
            B_slu = Buf("slu"); B_hidT = [Buf("hidT0"), Buf("hidT1")]; B_yo = [Buf("yo0"), Buf("yo1")]
            B_psX = Buf("psX"); B_psG = [Buf("psG0"), Buf("psG1")]; B_psU = [Buf("psU0"), Buf("psU1")]; B_psO = [Buf(f"psO{i}") for i in range(2)]
            B_ybd = Buf("yb_d")

            def p3_A(j):
                sl = j % 2
                w3 = (j // 2) % 3
                for (wt, wsrc, nm) in (((wg, wegb_d, "wg"), (wu, weub_d, "wu"), (wd, wedb_d, "wd")) if j % 2 == 0 else ()):
                    sc.dma("pool", lambda e, j=j, w3=w3, wt=wt, wsrc=wsrc: e.indirect_dma_start(
                        out=wt[w3][:].rearrange("p a b -> p (a b)"), out_offset=None, in_=wsrc.ap(),
                        in_offset=bass.IndirectOffsetOnAxis(ap=widx[:, j:j + 1], axis=0)),
                        reads=[B_blk, B_wexp], writes=[B_wg[w3]], key=f"{nm}{w3}")
                sc.dma("pool", lambda e, j=j, sl=sl: e.indirect_dma_start(
                    out=xgat[sl][:], out_offset=None, in_=n2_d.ap(),
                    in_offset=bass.IndirectOffsetOnAxis(ap=tokidx[:, j:j + 1], axis=0)),
                    reads=[B_tokidx, B_n2d], writes=[B_xgat[sl]], key=f"xgat{sl}")
                for k in range(8):
                    pxt = psXa if k < 4 else psXb
                    sc.op("pe", lambda e, k=k, sl=sl, pxt=pxt: e.transpose(pxt[:, (k % 4) * 128:(k % 4 + 1) * 128], xgat[sl][:, k::8], ident_b[:]),
                          reads=[B_xgat[sl], B_const], writes=[B_psX])
                sc.op("act", lambda e, sl=sl: e.activation(out=nTb[sl][:, 0:4, :], in_=psXa[:].rearrange("p (a b) -> p a b", a=4), func=AF.Copy),
                      reads=[B_psX], writes=[B_nTb[sl]])
                sc.op("dve", lambda e, sl=sl: e.tensor_copy(out=nTb[sl][:, 4:8, :], in_=psXb[:].rearrange("p (a b) -> p a b", a=4)),
                      reads=[B_psX], writes=[B_nTb[sl]])
                for hc in range(4):
                    for k in range(8):
                        sc.op("pe", lambda e, hc=hc, k=k, sl=sl, w3=w3: e.matmul(psG[sl][:, hc * 128:(hc + 1) * 128], lhsT=wg[w3][:, k, hc::4],
                                                                        rhs=nTb[sl][:, k, :], start=(k == 0), stop=(k == 7)),
                              reads=[B_wg[w3], B_nTb[sl]], writes=[B_psG[sl]])
                for hc in range(4):
                    for k in range(8):
                        sc.op("pe", lambda e, hc=hc, k=k, sl=sl, w3=w3: e.matmul(psU[sl][:, hc * 128:(hc + 1) * 128], lhsT=wu[w3][:, k, hc::4],
                                                                        rhs=nTb[sl][:, k, :], start=(k == 0), stop=(k == 7)),
                              reads=[B_wg[w3], B_nTb[sl]], writes=[B_psU[sl]])
                sc.op("act", lambda e, sl=sl: e.activation(out=slu[:], in_=psG[sl][:], func=AF.Silu), reads=[B_psG[sl]], writes=[B_slu])
                sc.op("dve", lambda e, sl=sl: e.tensor_tensor(out=hidT[sl][:].rearrange("p a b -> p (a b)"), in0=slu[:], in1=psU[sl][:], op=ALU.mult),
                      reads=[B_slu, B_psU[sl]], writes=[B_hidT[sl]])

            def p3_B(j):
                sl = j % 2
                w3 = (j // 2) % 3
                for half in range(2):
                    po = psO[half]
                    bpo = B_psO[half]
                    for hc in range(4):
                        sc.op("pe", lambda e, po=po, hc=hc, half=half, sl=sl, w3=w3: e.matmul(po[:], lhsT=hidT[sl][:, hc, :],
                                                                                     rhs=wd[w3][:, hc, half * 512:(half + 1) * 512],
                                                                                     start=(hc == 0), stop=(hc == 3)),
                              reads=[B_hidT[sl], B_wg[w3]], writes=[bpo])
                sc.op("act", lambda e, sl=sl: e.activation(out=yo[sl][:, 0:512], in_=psO[0][:], func=AF.Copy), reads=[B_psO[0]], writes=[B_yo[sl]])
                sc.op("dve", lambda e, sl=sl: e.tensor_copy(out=yo[sl][:, 512:1024], in_=psO[1][:]), reads=[B_psO[1]], writes=[B_yo[sl]])
                sc.dma("sp", lambda e, j=j, sl=sl: e.dma_start(out=yb_d.ap()[j * 128:(j + 1) * 128, :], in_=yo[sl][:]),
                       reads=[B_yo[sl]], writes=[B_ybd], key=f"ybst{sl}")

            p3_A(0)
            for j in range(NBLK):
                if j + 1 < NBLK:
                    p3_A(j + 1)
                p3_B(j)
            sc.barrier()
            sc.emit(final=(stop == 'p3'))
            if stop == 'p3':
                return

        p4 = ExitStack()
        with p4:
            Q = p4.enter_context
            r1 = [Q(nc.sbuf_tensor(f"r1_{i}", [128, D], F32)) for i in range(2)]
            r2 = [Q(nc.sbuf_tensor(f"r2_{i}", [128, D], F32)) for i in range(2)]
            h1t = [Q(nc.sbuf_tensor(f"h1t{i}", [128, D], F32)) for i in range(2)]
            ot = [Q(nc.sbuf_tensor(f"ot{i}", [128, D], F32)) for i in range(2)]
            junk4 = Q(nc.sbuf_tensor("junk4", [128, D], F32))
            gtf_row = Q(nc.sbuf_tensor("gtf_row", [128, D], F32))
            gfin_row = Q(nc.sbuf_tensor("gfin_row", [128, D], F32))
            sc.dma("sp", lambda e: e.dma_start(out=gfin_row[:], in_=bass.AP(tensor=gfin_d.ap().tensor, offset=0, ap=[[0, 128], [1, D]])),
                   writes=[B_rows], key="c_gfin")
            sc.dma("sp", lambda e: e.dma_start(out=gtf_row[:], in_=bass.AP(tensor=mod_d.ap().tensor, offset=5 * D, ap=[[0, 128], [1, D]])),
                   writes=[B_rows], key="c_gtf")
            st4 = Q(nc.sbuf_tensor("st4", [128, 2 * NT + 2], F32))
            B_r1 = [Buf("r1_0"), Buf("r1_1")]; B_r2 = [Buf("r2_0"), Buf("r2_1")]; B_h1t = [Buf("h1t0"), Buf("h1t1")]
            B_ot = [Buf("ot0"), Buf("ot1")]; B_j4 = Buf("junk4"); B_st4 = Buf("st4")
            B_out = Buf("out")
            sc.op("dve", lambda e: e.memset(st4[:], 0.0), writes=[B_st4])
            def p4_A(T):
                sl = T % 2
                sc.dma("pool", lambda e, T=T, sl=sl: e.indirect_dma_start(
                    out=r1[sl][:], out_offset=None, in_=yb_d.ap(),
                    in_offset=bass.IndirectOffsetOnAxis(ap=dest_i[:, T, 0:1], axis=0)),
                    reads=[B_dest, B_ybd], writes=[B_r1[sl]], key=f"r1_{sl}")
                sc.dma("pool", lambda e, T=T, sl=sl: e.indirect_dma_start(
                    out=r2[sl][:], out_offset=None, in_=yb_d.ap(),
                    in_offset=bass.IndirectOffsetOnAxis(ap=dest_i[:, T, 1:2], axis=0)),
                    reads=[B_dest, B_ybd], writes=[B_r2[sl]], key=f"r2_{sl}")
                sc.dma("sp", lambda e, T=T, sl=sl: e.dma_start(out=h1t[sl][:], in_=h1_d.ap()[T * 128:(T + 1) * 128, :]),
                       reads=[B_h1d], writes=[B_h1t[sl]], key=f"h1ld{sl}")
                sc.op("act", lambda e, T=T, sl=sl: e.activation(out=r1[sl][:], in_=r1[sl][:], func=AF.Copy, scale=tw[:, T, 0:1]),
                      reads=[B_r1[sl], B_tw], writes=[B_r1[sl]])
                sc.op("dve", lambda e, T=T, sl=sl: e.scalar_tensor_tensor(out=r2[sl][:], in0=r2[sl][:], scalar=tw[:, T, 1:2], in1=r1[sl][:],
                                                                        op0=ALU.mult, op1=ALU.add),
                      reads=[B_r2[sl], B_r1[sl], B_tw], writes=[B_r2[sl]])
                sc.op("dve", lambda e, sl=sl: e.tensor_tensor(out=r2[sl][:], in0=r2[sl][:], in1=gtf_row[:], op=ALU.mult),
                      reads=[B_r2[sl], B_rows], writes=[B_r2[sl]])
                sc.op("dve", lambda e, sl=sl: e.tensor_tensor(out=h1t[sl][:], in0=h1t[sl][:], in1=r2[sl][:], op=ALU.add),
                      reads=[B_r2[sl], B_h1t[sl]], writes=[B_h1t[sl]])
                sc.op("act", lambda e, T=T, sl=sl: e.activation(out=junk4[:], in_=h1t[sl][:], func=AF.Square, accum_out=st4[:, 2 * T:2 * T + 1]),
                      reads=[B_h1t[sl], B_st4], writes=[B_j4, B_st4])

            def p4_B(T):
                sl = T % 2
                sc.op("act", lambda e, T=T: e.activation(out=st4[:, 2 * T + 1:2 * T + 2], in_=st4[:, 2 * T:2 * T + 1], func=AF.Sqrt, scale=1.0 / D, bias=EPS),
                      reads=[B_st4], writes=[B_st4])
                sc.op("dve", lambda e, T=T: e.reciprocal(out=st4[:, 2 * T:2 * T + 1], in_=st4[:, 2 * T + 1:2 * T + 2]), reads=[B_st4], writes=[B_st4])
                sc.op("act", lambda e, T=T, sl=sl: e.activation(out=ot[sl][:], in_=h1t[sl][:], func=AF.Copy, scale=st4[:, 2 * T:2 * T + 1]),
                      reads=[B_h1t[sl], B_st4], writes=[B_ot[sl]])
                sc.op("dve", lambda e, sl=sl: e.tensor_tensor(out=ot[sl][:], in0=ot[sl][:], in1=gfin_row[:], op=ALU.mult),
                      reads=[B_ot[sl], B_rows], writes=[B_ot[sl]])
                sc.dma("sp", lambda e, T=T, sl=sl: e.dma_start(out=out_d.ap()[T * 128:(T + 1) * 128, :], in_=ot[sl][:]),
                       reads=[B_ot[sl]], writes=[B_out], key=f"ost{sl}")

            p4_A(0)
            for T in range(NT):
                if T + 1 < NT:
                    p4_A(T + 1)
                p4_B(T)
            sc.emit(final=True)


_NC_CACHE = {}


def _get_nc(debug=False):
    if debug not in _NC_CACHE:
        _NC_CACHE[debug] = build_program(debug=debug)
    return _NC_CACHE[debug]


def make_in_maps(inputs, cores):
    f = lambda a: np.ascontiguousarray(np.asarray(a, dtype=np.float32))
    x = f(inputs["x"])
    c = f(inputs["c"])
    shared = {
        "gmix2": np.ascontiguousarray(f(inputs["g_mix"]).reshape(8, 128).T),
        "g_ffn": f(inputs["g_ffn"]).reshape(1, D),
        "g_final": f(inputs["g_final"]).reshape(1, D),
        "w_ada": f(inputs["w_ada"]).reshape(D, 6 * D),
        "b_ada": f(inputs["b_ada"]).reshape(1, 6 * D),
        "w_in": f(inputs["w_in"]).reshape(D, 4608),
        "w_sba_out": f(inputs["w_sba_out"]).reshape(512, D),
        "g_sgu": f(inputs["g_sgu"]).reshape(1, 512),
        "w_spatial": f(inputs["w_spatial"]).reshape(8, 128, 128),
        "b_spatial": f(inputs["b_spatial"]).reshape(8, 128),
        "w_sgu_out": f(inputs["w_sgu_out"]).reshape(512, D),
        "w_out": f(inputs["w_out"]).reshape(D, D),
        "w_r": np.ascontiguousarray(np.concatenate([f(inputs["w_router_group"]).reshape(D, 4),
                                                    f(inputs["w_router_expert"]).reshape(D, 32)], axis=1)),
        "b_r": np.ascontiguousarray(np.concatenate([f(inputs["b_router_group"]).reshape(1, 4),
                                                    f(inputs["b_router_expert"]).reshape(1, 32)], axis=1)),
        "w_eg": f(inputs["w_expert_gate"]).reshape(32, D, 512),
        "w_eu": f(inputs["w_expert_up"]).reshape(32, D, 512),
        "w_ed": f(inputs["w_expert_down"]).reshape(32, 512, D),
    }
    maps = []
    for b in cores:
        m = dict(shared)
        m["x"] = np.ascontiguousarray(x[b])
        m["c2"] = np.ascontiguousarray(c[b].reshape(8, 128).T)
        maps.append(m)
    return maps


def kernel(**inputs):
    nc = _get_nc(False)
    in_maps = make_in_maps(inputs, list(range(8)))
    res = run_bass_kernel_spmd(nc, in_maps, core_ids=list(range(8)))
    return np.stack([np.asarray(r["out"], dtype=np.float32) for r in res.results], axis=0)
```
